# Optimizing a Trainium2 kernel written in Bass

```python
import jax, jax.numpy as jnp
from jax import lax
import numpy as np

D_MODEL = 1024
BATCH = 8
SEQ = 2048
DEPTH = 2

HEAD_DIM = 64
ROPE_THETA = 10000.0
NORM_EPS = 1e-6
N_BRANCH = 4
A_WIDTH = 512
A_CONV = 3
B_WIDTH = 512
B_CONV = 31
C_HEADS = 8
C_KV_HEADS = 2
C_GROUP = C_HEADS // C_KV_HEADS
C_WIDTH = C_HEADS * HEAD_DIM
C_KV_WIDTH = C_KV_HEADS * HEAD_DIM
CMP_BLOCK = 32
CMP_STRIDE = 16
CMP_HIDDEN = 128
SLC_BLOCK = 64
SLC_TOPN = 16
SLC_QCHUNK = 32
WIN = 512
WIN_QBLOCK = 128
D_HEADS = 8
D_WIDTH = D_HEADS * HEAD_DIM
MOBA_BLOCK = 256
MOBA_TOPK = 3
MOBA_QCHUNK = 16
PEER_HEADS = 8
PEER_NKEYS = 128
PEER_EXPERTS = PEER_NKEYS * PEER_NKEYS
PEER_QDIM = 256
PEER_TOPK = 16
PEER_TCHUNK = 128

IN_SIZES = (A_WIDTH, A_WIDTH, A_WIDTH, B_WIDTH, B_WIDTH, C_WIDTH) + (C_KV_WIDTH,) * 6 + (3 * C_HEADS, D_WIDTH, D_WIDTH, D_WIDTH, N_BRANCH * D_MODEL)
IN_COLS = sum(IN_SIZES)
IN_SPLITS = [int(v) for v in np.cumsum(IN_SIZES)[:-1]]

kernel_name = 'hybrid_conv_nsa_moba_peer_block'


def rms_norm(x, g):
    xf = x.astype(jnp.float32)
    y = xf * lax.rsqrt(jnp.mean(xf * xf, axis=-1, keepdims=True) + NORM_EPS)
    return (y * g.astype(jnp.float32)).astype(x.dtype)


def layer_norm(x, g, b):
    xf = x.astype(jnp.float32)
    mu = jnp.mean(xf, axis=-1, keepdims=True)
    var = jnp.mean(jnp.square(xf - mu), axis=-1, keepdims=True)
    y = (xf - mu) * lax.rsqrt(var + NORM_EPS)
    return (y * g.astype(jnp.float32) + b.astype(jnp.float32)).astype(x.dtype)


def rope_tables(positions):
    inv = 1.0 / (ROPE_THETA ** (jnp.arange(0, HEAD_DIM, 2, dtype=jnp.float32) / HEAD_DIM))
    ang = positions.astype(jnp.float32)[..., None] * inv
    return jnp.cos(ang)[:, :, None, :], jnp.sin(ang)[:, :, None, :]


def apply_rope(x, cos, sin):
    xf = x.astype(jnp.float32)
    x1, x2 = jnp.split(xf, 2, axis=-1)
    return jnp.concatenate([x1 * cos - x2 * sin, x2 * cos + x1 * sin], axis=-1).astype(x.dtype)


def causal_dwconv(x, w):
    k, ch = w.shape
    return lax.conv_general_dilated(x, w[:, None, :].astype(x.dtype), window_strides=(1,), padding=[(k - 1, 0)], dimension_numbers=('NWC', 'WIO', 'NWC'), feature_group_count=ch)


def masked_softmax(s, mask, axis):
    s = jnp.where(mask, s.astype(jnp.float32), -jnp.inf)
    m = jnp.max(s, axis=axis, keepdims=True)
    m = jnp.where(jnp.isfinite(m), m, 0.0)
    e = jnp.where(mask, jnp.exp(s - m), 0.0)
    return e / jnp.maximum(jnp.sum(e, axis=axis, keepdims=True), 1e-30)


def short_conv_mixer(a_b, a_c, a_x, conv_w, w_out):
    return (a_b * causal_dwconv(a_c * a_x, conv_w)) @ w_out


def conformer_conv_module(b_a, b_g, conv_w, conv_b, ln_g, ln_b, w_out):
    u = b_a * jax.nn.sigmoid(b_g)
    u = causal_dwconv(u, conv_w) + conv_b
    u = jax.nn.silu(layer_norm(u, ln_g, ln_b))
    return u @ w_out


def nsa_compress(k, pos, w1, w2):
    bsz, s = k.shape[:2]
    n_cmp = (s - CMP_BLOCK) // CMP_STRIDE + 1
    idx = jnp.arange(n_cmp)[:, None] * CMP_STRIDE + jnp.arange(CMP_BLOCK)[None, :]
    blk = k[:, idx] + pos[None, None, :, None, :]
    blk = blk.transpose(0, 1, 3, 2, 4).reshape(bsz, n_cmp, C_KV_HEADS, CMP_BLOCK * HEAD_DIM)
    return jax.nn.gelu(blk @ w1, approximate=False) @ w2


def nsa_attention(q, kc, vc, ks, vs, kw, vw, gate_logits, cmp_pos, cmp_w1, cmp_w2, cos, sin, w_out):
    bsz, s = q.shape[:2]
    scale = HEAD_DIM ** -0.5
    t = jnp.arange(s)
    q = apply_rope(q.reshape(bsz, s, C_HEADS, HEAD_DIM), cos, sin).reshape(bsz, s, C_KV_HEADS, C_GROUP, HEAD_DIM)
    heads = lambda z: z.reshape(bsz, s, C_KV_HEADS, HEAD_DIM)
    kc = apply_rope(heads(kc), cos, sin)
    ks = apply_rope(heads(ks), cos, sin)
    kw = apply_rope(heads(kw), cos, sin)
    vc, vs, vw = heads(vc), heads(vs), heads(vw)

    k_cmp = nsa_compress(kc, cmp_pos[0], cmp_w1[0], cmp_w2[0])
    v_cmp = nsa_compress(vc, cmp_pos[1], cmp_w1[1], cmp_w2[1])
    n_cmp = k_cmp.shape[1]
    cmp_start = jnp.arange(n_cmp) * CMP_STRIDE
    s_cmp = jnp.einsum('bsgrd,bngd->bgrsn', q, k_cmp) * scale
    p_cmp = masked_softmax(s_cmp, (cmp_start + CMP_BLOCK - 1)[None, :] <= t[:, None], -1)
    o_cmp = jnp.einsum('bgrsn,bngd->bsgrd', p_cmp.astype(v_cmp.dtype), v_cmp)

    n_slc = s // SLC_BLOCK
    top_n = min(SLC_TOPN, n_slc)
    slc_start = jnp.arange(n_slc) * SLC_BLOCK
    overlap = ((cmp_start[:, None] < slc_start[None, :] + SLC_BLOCK) & (cmp_start[:, None] + CMP_BLOCK > slc_start[None, :])).astype(jnp.float32)
    imp = jnp.einsum('bgrsn,nj->bgsj', p_cmp, overlap)
    cur = t // SLC_BLOCK
    j = jnp.arange(n_slc)
    forced = (j[None, :] == 0) | (j[None, :] == cur[:, None]) | (j[None, :] == cur[:, None] - 1)
    imp = jnp.where(forced, jnp.inf, jnp.where(slc_start[None, :] > t[:, None], -jnp.inf, imp))
    _, sel = lax.top_k(imp, top_n)

    def to_blocks(z):
        return z.reshape(bsz, n_slc, SLC_BLOCK, C_KV_HEADS, HEAD_DIM).transpose(0, 3, 1, 2, 4).reshape(bsz, C_KV_HEADS, n_slc, SLC_BLOCK * HEAD_DIM)

    ks_blk, vs_blk = to_blocks(ks), to_blocks(vs)
    nq = s // SLC_QCHUNK
    q_ch = jnp.moveaxis(q.reshape(bsz, nq, SLC_QCHUNK, C_KV_HEADS, C_GROUP, HEAD_DIM), 1, 0)
    sel_ch = jnp.moveaxis(sel.reshape(bsz, C_KV_HEADS, nq, SLC_QCHUNK, top_n), 2, 0)
    t_ch = t.reshape(nq, SLC_QCHUNK)

    def slc_chunk(args):
        qc, ic, tc = args
        flat = ic.reshape(bsz, C_KV_HEADS, SLC_QCHUNK * top_n, 1)
        shp = (bsz, C_KV_HEADS, SLC_QCHUNK, top_n, SLC_BLOCK, HEAD_DIM)
        kg = jnp.take_along_axis(ks_blk, flat, axis=2).reshape(shp)
        vg = jnp.take_along_axis(vs_blk, flat, axis=2).reshape(shp)
        sc = jnp.einsum('bqgrd,bgqnkd->bgrqnk', qc, kg) * scale
        kpos = ic[..., None] * SLC_BLOCK + jnp.arange(SLC_BLOCK)
        mask = (kpos <= tc[:, None, None])[:, :, None]
        p = masked_softmax(sc, mask, (-2, -1))
        return jnp.einsum('bgrqnk,bgqnkd->bqgrd', p.astype(vg.dtype), vg)

    o_slc = jnp.moveaxis(lax.map(slc_chunk, (q_ch, sel_ch, t_ch)), 0, 1).reshape(bsz, s, C_KV_HEADS, C_GROUP, HEAD_DIM)

    nqb = s // WIN_QBLOCK
    nband = WIN // WIN_QBLOCK + 1

    def band(z):
        zp = jnp.pad(z, ((0, 0), (WIN, 0), (0, 0), (0, 0))).reshape(bsz, nqb + nband - 1, WIN_QBLOCK, C_KV_HEADS, HEAD_DIM)
        return jnp.concatenate([zp[:, i:i + nqb] for i in range(nband)], axis=2)

    kb, vb = band(kw), band(vw)
    qb = q.reshape(bsz, nqb, WIN_QBLOCK, C_KV_HEADS, C_GROUP, HEAD_DIM)
    sc = jnp.einsum('bnqgrd,bnkgd->bngrqk', qb, kb) * scale
    tq = t.reshape(nqb, WIN_QBLOCK)[:, :, None]
    kpos = (jnp.arange(nqb)[:, None] * WIN_QBLOCK - WIN + jnp.arange(nband * WIN_QBLOCK)[None, :])[:, None, :]
    mask = (kpos >= 0) & (kpos <= tq) & (kpos > tq - WIN)
    p = masked_softmax(sc, mask[None, :, None, None], -1)
    o_win = jnp.einsum('bngrqk,bnkgd->bnqgrd', p.astype(vb.dtype), vb).reshape(bsz, s, C_KV_HEADS, C_GROUP, HEAD_DIM)

    g = jax.nn.sigmoid(gate_logits).reshape(bsz, s, 3, C_KV_HEADS, C_GROUP, 1)
    o = g[:, :, 0] * o_cmp + g[:, :, 1] * o_slc + g[:, :, 2] * o_win
    return o.reshape(bsz, s, C_WIDTH) @ w_out


def moba_attention(q, k, v, cos, sin, w_out):
    bsz, s = q.shape[:2]
    scale = HEAD_DIM ** -0.5
    heads = lambda z: z.reshape(bsz, s, D_HEADS, HEAD_DIM)
    q = apply_rope(heads(q), cos, sin)
    k = apply_rope(heads(k), cos, sin)
    v = heads(v)
    nb = -(-s // MOBA_BLOCK)
    pad = nb * MOBA_BLOCK - s
    kp = jnp.pad(k, ((0, 0), (0, pad), (0, 0), (0, 0))).reshape(bsz, nb, MOBA_BLOCK, D_HEADS, HEAD_DIM)
    vp = jnp.pad(v, ((0, 0), (0, pad), (0, 0), (0, 0))).reshape(bsz, nb, MOBA_BLOCK, D_HEADS, HEAD_DIM)
    t = jnp.arange(s)
    own = t // MOBA_BLOCK
    kmean = jnp.mean(kp.astype(jnp.float32), axis=2).astype(q.dtype)
    gate = jnp.einsum('bshd,bnhd->bhsn', q, kmean)
    past = jnp.arange(nb)[None, :] < own[:, None]
    gate = jnp.where(past, gate.astype(jnp.float32), -jnp.inf)
    n_pick = min(MOBA_TOPK, nb)
    gval, gidx = lax.top_k(gate, n_pick)
    own_idx = jnp.broadcast_to(own[None, None, :, None], (bsz, D_HEADS, s, 1)).astype(gidx.dtype)
    idx = jnp.concatenate([gidx, own_idx], axis=-1)
    valid = jnp.concatenate([jnp.isfinite(gval), jnp.ones(own_idx.shape, dtype=bool)], axis=-1)
    n_sel = n_pick + 1
    kblk = kp.transpose(0, 3, 1, 2, 4).reshape(bsz, D_HEADS, nb, MOBA_BLOCK * HEAD_DIM)
    vblk = vp.transpose(0, 3, 1, 2, 4).reshape(bsz, D_HEADS, nb, MOBA_BLOCK * HEAD_DIM)
    nq = s // MOBA_QCHUNK
    q_ch = jnp.moveaxis(q.reshape(bsz, nq, MOBA_QCHUNK, D_HEADS, HEAD_DIM), 1, 0)
    idx_ch = jnp.moveaxis(idx.reshape(bsz, D_HEADS, nq, MOBA_QCHUNK, n_sel), 2, 0)
    val_ch = jnp.moveaxis(valid.reshape(bsz, D_HEADS, nq, MOBA_QCHUNK, n_sel), 2, 0)
    t_ch = t.reshape(nq, MOBA_QCHUNK)

    def moba_chunk(args):
        qc, ic, vc, tc = args
        flat = ic.reshape(bsz, D_HEADS, MOBA_QCHUNK * n_sel, 1)
        shp = (bsz, D_HEADS, MOBA_QCHUNK, n_sel, MOBA_BLOCK, HEAD_DIM)
        kg = jnp.take_along_axis(kblk, flat, axis=2).reshape(shp)
        vg = jnp.take_along_axis(vblk, flat, axis=2).reshape(shp)
        sc = jnp.einsum('bqhd,bhqnkd->bhqnk', qc, kg) * scale
        kpos = ic[..., None] * MOBA_BLOCK + jnp.arange(MOBA_BLOCK)
        mask = vc[..., None] & (kpos <= tc[:, None, None])
        p = masked_softmax(sc, mask, (-2, -1))
        return jnp.einsum('bhqnk,bhqnkd->bqhd', p.astype(vg.dtype), vg)

    o = jnp.moveaxis(lax.map(moba_chunk, (q_ch, idx_ch, val_ch, t_ch)), 0, 1).reshape(bsz, s, D_WIDTH)
    return o @ w_out


def peer_ffn(h, wq, subkeys, u, v):
    bsz, s, d = h.shape
    hf = h.reshape(bsz * s, d)
    q = (hf @ wq).reshape(-1, PEER_HEADS, 2, PEER_QDIM // 2)
    qf = q.astype(jnp.float32)
    q = (qf * lax.rsqrt(jnp.mean(qf * qf, axis=-1, keepdims=True) + NORM_EPS)).astype(h.dtype)
    sc = jnp.einsum('thpd,hpnd->thpn', q, subkeys).astype(jnp.float32)
    v1, i1 = lax.top_k(sc[:, :, 0], PEER_TOPK)
    v2, i2 = lax.top_k(sc[:, :, 1], PEER_TOPK)
    cand = (v1[..., :, None] + v2[..., None, :]).reshape(-1, PEER_HEADS, PEER_TOPK * PEER_TOPK)
    top, ci = lax.top_k(cand, PEER_TOPK)
    expert = jnp.take_along_axis(i1, ci // PEER_TOPK, axis=-1) * PEER_NKEYS + jnp.take_along_axis(i2, ci % PEER_TOPK, axis=-1)
    g = jax.nn.softmax(top, axis=-1)
    nch = hf.shape[0] // PEER_TCHUNK

    def peer_chunk(args):
        hc, ec, gc = args
        ue = jnp.take(u, ec, axis=0)
        act = jax.nn.gelu(jnp.einsum('td,thkd->thk', hc, ue), approximate=False)
        w = (gc * act).astype(v.dtype)
        return jnp.einsum('thk,thkd->td', w, jnp.take(v, ec, axis=0))

    out = lax.map(peer_chunk, (hf.reshape(nch, PEER_TCHUNK, d), expert.reshape(nch, PEER_TCHUNK, PEER_HEADS, PEER_TOPK), g.reshape(nch, PEER_TCHUNK, PEER_HEADS, PEER_TOPK)))
    return out.reshape(bsz, s, d)


def setup_inputs(seed: int = 0) -> dict:
    key = jax.random.key(seed)
    ks = jax.random.split(key, 32)

    def nrm(k, shape, scale):
        return scale * jax.random.normal(k, shape, jnp.float32)

    L = DEPTH
    return {
        'x': nrm(ks[0], (BATCH, SEQ, D_MODEL), 1.0),
        'c': nrm(ks[1], (BATCH, D_MODEL), 1.0),
        'positions': jax.random.randint(ks[2], (BATCH, 1), 0, 4096, dtype=jnp.int32) + jnp.arange(SEQ, dtype=jnp.int32)[None, :],
        'mod_w': nrm(ks[3], (L, D_MODEL, 6 * D_MODEL), 0.5 * D_MODEL ** -0.5),
        'mod_b': nrm(ks[4], (L, 6 * D_MODEL), 0.02),
        'norm_mix_g': 1.0 + nrm(ks[5], (L, D_MODEL), 0.05),
        'norm_ffn_g': 1.0 + nrm(ks[6], (L, D_MODEL), 0.05),
        'w_in': nrm(ks[7], (L, D_MODEL, IN_COLS), D_MODEL ** -0.5),
        'a_conv_w': nrm(ks[8], (L, A_CONV, A_WIDTH), A_CONV ** -0.5),
        'a_out': nrm(ks[9], (L, A_WIDTH, D_MODEL), A_WIDTH ** -0.5),
        'b_conv_w': nrm(ks[10], (L, B_CONV, B_WIDTH), B_CONV ** -0.5),
        'b_conv_b': nrm(ks[11], (L, B_WIDTH), 0.02),
        'b_ln_g': 1.0 + nrm(ks[12], (L, B_WIDTH), 0.05),
        'b_ln_b': nrm(ks[13], (L, B_WIDTH), 0.02),
        'b_out': nrm(ks[14], (L, B_WIDTH, D_MODEL), B_WIDTH ** -0.5),
        'c_cmp_pos': nrm(ks[15], (L, 2, CMP_BLOCK, HEAD_DIM), 0.1),
        'c_cmp_w1': nrm(ks[16], (L, 2, CMP_BLOCK * HEAD_DIM, CMP_HIDDEN), (CMP_BLOCK * HEAD_DIM) ** -0.5),
        'c_cmp_w2': nrm(ks[17], (L, 2, CMP_HIDDEN, HEAD_DIM), CMP_HIDDEN ** -0.5),
        'c_out': nrm(ks[18], (L, C_WIDTH, D_MODEL), C_WIDTH ** -0.5),
        'd_out': nrm(ks[19], (L, D_WIDTH, D_MODEL), D_WIDTH ** -0.5),
        'w_o': nrm(ks[20], (L, D_MODEL, D_MODEL), D_MODEL ** -0.5),
        'peer_wq': nrm(ks[21], (L, D_MODEL, PEER_HEADS * PEER_QDIM), D_MODEL ** -0.5),
        'peer_subkeys': nrm(ks[22], (L, PEER_HEADS, 2, PEER_NKEYS, PEER_QDIM // 2), (PEER_QDIM // 2) ** -0.5),
        'peer_u': nrm(ks[23], (L, PEER_EXPERTS, D_MODEL), D_MODEL ** -0.5),
        'peer_v': nrm(ks[24], (L, PEER_EXPERTS, D_MODEL), PEER_HEADS ** -0.5),
        'final_norm_g': 1.0 + nrm(ks[25], (D_MODEL,), 0.05),
    }


def reference(x, c, positions, mod_w, mod_b, norm_mix_g, norm_ffn_g, w_in, a_conv_w, a_out, b_conv_w, b_conv_b, b_ln_g, b_ln_b, b_out, c_cmp_pos, c_cmp_w1, c_cmp_w2, c_out, d_out, w_o, peer_wq, peer_subkeys, peer_u, peer_v, final_norm_g):
    bsz, s, d = x.shape
    cos, sin = rope_tables(positions)
    cond = jax.nn.silu(c)
    for l in range(DEPTH):
        mod = (cond @ mod_w[l] + mod_b[l])[:, None, :]
        sh1, sc1, gt1, sh2, sc2, gt2 = jnp.split(mod, 6, axis=-1)
        h = rms_norm(x, norm_mix_g[l]) * (1 + sc1) + sh1
        (a_b, a_c, a_x, b_a, b_g, c_q, c_kc, c_vc, c_ks, c_vs, c_kw, c_vw, c_gate, d_q, d_k, d_v, merge) = jnp.split(h @ w_in[l], IN_SPLITS, axis=-1)
        y_a = short_conv_mixer(a_b, a_c, a_x, a_conv_w[l], a_out[l])
        y_b = conformer_conv_module(b_a, b_g, b_conv_w[l], b_conv_b[l], b_ln_g[l], b_ln_b[l], b_out[l])
        y_c = nsa_attention(c_q, c_kc, c_vc, c_ks, c_vs, c_kw, c_vw, c_gate, c_cmp_pos[l], c_cmp_w1[l], c_cmp_w2[l], cos, sin, c_out[l])
        y_d = moba_attention(d_q, d_k, d_v, cos, sin, d_out[l])
        gates = jax.nn.sigmoid(merge).reshape(bsz, s, N_BRANCH, d)
        merged = gates[:, :, 0] * y_a + gates[:, :, 1] * y_b + gates[:, :, 2] * y_c + gates[:, :, 3] * y_d
        x = x + gt1 * (merged @ w_o[l])
        h2 = rms_norm(x, norm_ffn_g[l]) * (1 + sc2) + sh2
        x = x + gt2 * peer_ffn(h2, peer_wq[l], peer_subkeys[l], peer_u[l], peer_v[l])
    return rms_norm(x, final_norm_g)
```

```python
import os
import numpy as np
import ml_dtypes
import concourse.bass as bass
import concourse.mybir as mybir
from concourse.bass_utils import run_bass_kernel_spmd

F32 = mybir.dt.float32
BF16 = mybir.dt.bfloat16
I32 = mybir.dt.int32
U32 = mybir.dt.uint32
U8 = mybir.dt.uint8
ALU = mybir.AluOpType
AF = mybir.ActivationFunctionType
AX = mybir.AxisListType
_DTSZ = {F32: 4, BF16: 2, I32: 4, U32: 4, U8: 1}
NPBF = ml_dtypes.bfloat16

SEQ = 2048
DM = 1024
DEPTH = 2
NCORES = 8
BIG = 1.0e30
NEGB = 30000.0


def _box(ap):
    t = ap.tensor
    sz = _DTSZ[ap.dtype]
    pairs = list(ap.ap)
    off = int(ap.offset)
    if type(t).__name__ == "DRamTensorHandle":
        span = 1
        for st, cnt in pairs:
            span += (cnt - 1) * abs(st)
        return (t.name, 0, 1, off * sz, (off + span) * sz)
    pstride = 1
    for s in list(t.shape)[1:]:
        pstride *= s
    p0 = off // pstride
    f0 = off % pstride
    if pairs and pairs[0][0] == pstride:
        pc = pairs[0][1]
        rest = pairs[1:]
    else:
        pc = 1
        rest = pairs
    span = 1
    for st, cnt in rest:
        span += (cnt - 1) * abs(st)
    return (t.name, p0, p0 + pc, f0 * sz, (f0 + span) * sz)


class Sched:
    ENG = ("pe", "act", "dve", "pool", "sp")

    def __init__(self, nc, n_dma_slots=8):
        self.nc = nc
        self.ops = []
        self.rec = {}
        self.n_dma_slots = n_dma_slots

    def emit(self, eng, fn, w=(), r=(), dma=False):
        oid = len(self.ops)
        deps = set()
        for ap in r:
            self._access(oid, eng, dma, ap, False, deps)
        for ap in w:
            self._access(oid, eng, dma, ap, True, deps)
        deps.discard(oid)
        self.ops.append(dict(eng=eng, fn=fn, deps=deps, dma=dma))
        return oid

    def _access(self, oid, eng, dma, ap, is_write, deps):
        name, p0, p1, b0, b1 = _box(ap)
        lst = self.rec.get(name)
        if lst is None:
            lst = self.rec[name] = []
        keep = []
        for rc in lst:
            q0, q1, c0, c1, rid, rw, reng, rdma = rc
            if q1 <= p0 or p1 <= q0 or c1 <= b0 or b1 <= c0 or rid == oid:
                keep.append(rc)
                continue
            if is_write or rw:
                deps.add(rid)
            covered = (p0 <= q0 and q1 <= p1 and b0 <= c0 and c1 <= b1)
            if is_write and covered:
                continue
            if (not is_write) and (not rw) and covered and reng == eng and not dma and not rdma:
                continue
            keep.append(rc)
        keep.append((p0, p1, b0, b1, oid, is_write, eng, dma))
        self.rec[name] = keep

    def finalize(self):
        nc = self.nc
        ops = self.ops
        needed = set()
        for o in ops:
            nd = set()
            for d in o["deps"]:
                po = ops[d]
                if po["eng"] == "pe" and o["eng"] == "pe" and not po["dma"] and not o["dma"]:
                    continue
                nd.add(d)
            o["deps"] = nd
            needed |= nd
        engobj = dict(pe=nc.tensor, act=nc.scalar, dve=nc.vector, pool=nc.gpsimd, sp=nc.sync)
        sems = {e: nc.alloc_semaphore("s_" + e) for e in self.ENG}
        dma_sems = {e: [nc.alloc_semaphore("d_%s%d" % (e, i)) for i in range(self.n_dma_slots)] for e in ("sp", "pool")}
        cnt = {e: 0 for e in self.ENG}
        slot_next = {e: 0 for e in dma_sems}
        slot_val = {e: [0] * self.n_dma_slots for e in dma_sems}
        waited = {e: {} for e in self.ENG}
        sig = {}
        n_wait = 0
        for i, o in enumerate(ops):
            e = o["eng"]
            eo = engobj[e]
            req = {}
            for d in o["deps"]:
                k, so, v = sig[d]
                if k not in req or req[k][1] < v:
                    req[k] = (so, v)
            if o["dma"]:
                s = slot_next[e]
                slot_next[e] = (s + 1) % self.n_dma_slots
                k = ("d", e, s)
                pv = slot_val[e][s]
                if pv > 0 and (k not in req or req[k][1] < pv):
                    req[k] = (dma_sems[e][s], pv)
            for k2, (so, v) in req.items():
                if waited[e].get(k2, 0) >= v:
                    continue
                eo.wait_ge(so, v)
                waited[e][k2] = v
                n_wait += 1
            inst = o["fn"](eo)
            if o["dma"]:
                slot_val[e][s] += 16
                inst.then_inc(dma_sems[e][s], 16)
                sig[i] = (k, dma_sems[e][s], slot_val[e][s])
            elif i in needed:
                cnt[e] += 1
                inst.then_inc(sems[e], 1)
                sig[i] = (("e", e), sems[e], cnt[e])
            o["fn"] = None
        eo = engobj["sp"]
        for e in dma_sems:
            for s in range(self.n_dma_slots):
                if slot_val[e][s] > 0:
                    eo.wait_ge(dma_sems[e][s], slot_val[e][s])
        for e in self.ENG:
            if cnt[e] > 0:
                eo.wait_ge(sems[e], cnt[e])
        return dict(n_ops=len(ops), n_wait=n_wait, cnt=dict(cnt))


SEC = dict(a_b=0, a_c=512, a_x=1024, b_a=1536, b_g=2048, c_q=2560, c_kc=3072, c_vc=3200, c_ks=3328,
           c_vs=3456, c_kw=3584, c_vw=3712, c_gate=3840, d_q=3864, d_k=4376, d_v=4888, merge=5400)


def _blocks():
    blk = {}
    cols = []

    def add(name, c0, n=128, swap=False):
        idx = np.arange(c0, c0 + 128)
        if n < 128:
            idx = np.where(np.arange(128) < n, idx, -1)
        if swap:
            idx = idx.reshape(2, 2, 32)[:, ::-1, :].reshape(128)
        blk[name] = len(cols)
        cols.append(idx)

    for s in ("a_b", "a_c", "a_x", "b_a", "b_g"):
        for j in range(4):
            add("%s%d" % (s, j), SEC[s] + 128 * j)
    for j in range(4):
        add("c_q%d" % j, SEC["c_q"] + 128 * j)
        add("c_q_sw%d" % j, SEC["c_q"] + 128 * j, swap=True)
    for s in ("c_kc", "c_ks", "c_kw"):
        add(s, SEC[s])
        add(s + "_sw", SEC[s], swap=True)
    for s in ("c_vc", "c_vs", "c_vw"):
        add(s, SEC[s])
    add("c_gate", SEC["c_gate"], n=24)
    for s in ("d_q", "d_k"):
        for j in range(4):
            add("%s%d" % (s, j), SEC[s] + 128 * j)
            add("%s_sw%d" % (s, j), SEC[s] + 128 * j, swap=True)
    for j in range(4):
        add("d_v%d" % j, SEC["d_v"] + 128 * j)
    for j in range(32):
        add("merge%d" % j, SEC["merge"] + 128 * j)
    return blk, np.stack(cols)


BLK, BLKCOLS = _blocks()
NBLK = BLKCOLS.shape[0]


def _consts():
    c = {}
    c["ident"] = np.eye(128, dtype=np.float32)
    kl = np.arange(128)[:, None]
    ql = np.arange(512)[None, :]
    cm = np.stack([(kl + 128 * o <= ql) for o in range(4)], axis=1).astype(np.float32)
    c["cmask"] = cm.astype(NPBF)
    c["wmask"] = (1.0 - cm).astype(NPBF)
    n = np.arange(128)[:, None]
    t = np.arange(SEQ)[None, :]
    c["cmpmask"] = ((16 * n + 31 <= t) & (n < 127)).astype(NPBF)
    j = np.arange(32)[None, :]
    cs = 16 * n
    ov = ((cs < 64 * j + 64) & (cs + 32 > 64 * j) & (n < 127)).astype(np.float32)
    c["overlap"] = ov
    tt = np.arange(SEQ)
    cur = tt // 64
    jj = np.arange(32)[None, :]
    forced = (jj == 0) | (jj == cur[:, None]) | (jj == cur[:, None] - 1)
    fut = jj > cur[:, None]
    add = np.where(forced, BIG, np.where(fut, -BIG, 0.0)).astype(np.float32)
    c["slcadd"] = np.ascontiguousarray(add.reshape(16, 128, 32).transpose(1, 0, 2))
    keys = np.arange(SEQ)[None, :]
    c["slcaug"] = (NEGB * (keys // 64 == np.arange(32)[:, None])).astype(NPBF)
    c["mobaug"] = (NEGB * (keys // 256 == np.arange(8)[:, None])).astype(NPBF)
    own = np.arange(8)[:, None]
    nn = np.arange(8)[None, :]
    ma = np.where(nn < own, 0.0, -BIG).astype(np.float32)
    c["mobadd"] = np.ascontiguousarray(np.broadcast_to(ma[None], (128, 8, 8))).astype(np.float32)
    c["notown"] = np.ascontiguousarray(np.broadcast_to((nn != own).astype(np.float32)[None], (128, 8, 8)))
    sel = np.zeros((32, 24, 64), np.float32)
    for i in range(24):
        sel[i, i, :] = 1.0
    c["selall"] = sel.astype(NPBF)
    c["iota"] = np.ascontiguousarray(np.broadcast_to(np.arange(128, dtype=np.float32)[None], (128, 128)))
    inv = 1.0 / (10000.0 ** (np.arange(0, 64, 2, dtype=np.float32) / np.float32(64)))
    inv = inv.astype(np.float32)
    c["invf"] = np.concatenate([inv, inv])[:, None].astype(np.float32)
    c["sgn"] = np.concatenate([-np.ones(32), np.ones(32)])[:, None].astype(np.float32)
    return c


CONSTS = _consts()


def _prep_shared(inp):
    d = {}
    L = DEPTH

    def fm(v, nk):
        return np.ascontiguousarray(v.reshape(nk, 128).T)

    w_in = inp["w_in"]
    wpad = np.concatenate([w_in, np.zeros((L, DM, 1), np.float32)], axis=2)
    cols = np.where(BLKCOLS < 0, w_in.shape[2], BLKCOLS)
    for l in range(L):
        g = wpad[l][:, cols]
        d["win%d" % l] = np.ascontiguousarray(g.reshape(8, 128, NBLK, 128).transpose(2, 1, 0, 3))
        mw = inp["mod_w"][l].reshape(8, 128, 12, 512).transpose(2, 1, 0, 3)
        d["modw%d" % l] = np.ascontiguousarray(mw)
        d["modbT%d" % l] = fm(inp["mod_b"][l], 48)
        d["modbrow%d" % l] = np.ascontiguousarray(inp["mod_b"][l][None, :])
        d["gmix%d" % l] = fm(inp["norm_mix_g"][l], 8)
        d["gffn%d" % l] = fm(inp["norm_ffn_g"][l], 8)
        d["acw%d" % l] = np.ascontiguousarray(inp["a_conv_w"][l].reshape(3, 4, 128).transpose(2, 1, 0))
        d["bcw%d" % l] = np.ascontiguousarray(inp["b_conv_w"][l].reshape(31, 4, 128).transpose(2, 1, 0))
        d["bcb%d" % l] = fm(inp["b_conv_b"][l], 4)
        d["blg%d" % l] = fm(inp["b_ln_g"][l], 4)
        d["blb%d" % l] = fm(inp["b_ln_b"][l], 4)
        for nm in ("a_out", "b_out"):
            w = inp[nm][l].reshape(4, 128, 8, 128).transpose(2, 1, 0, 3)
            d["%s%d" % (nm, l)] = np.ascontiguousarray(w)
        for nm in ("c_out", "d_out"):
            w = inp[nm][l].reshape(8, 64, 8, 128).transpose(2, 1, 0, 3)
            d["%s%d" % (nm, l)] = np.ascontiguousarray(w)
        d["wo%d" % l] = np.ascontiguousarray(inp["w_o"][l].reshape(8, 128, 1024).transpose(1, 0, 2))
        d["wq%d" % l] = np.ascontiguousarray(inp["peer_wq"][l].reshape(8, 128, 2048).transpose(1, 0, 2))
        sk = inp["peer_subkeys"][l].reshape(16, 128, 128)
        d["skT%d" % l] = np.ascontiguousarray(sk.transpose(2, 0, 1))
        d["pu%d" % l] = inp["peer_u"][l]
        d["pv%d" % l] = inp["peer_v"][l]
        w1 = inp["c_cmp_w1"][l].reshape(2, 32, 64, 128).transpose(2, 0, 1, 3)
        d["w1r%d" % l] = np.ascontiguousarray(w1)
        d["w2r%d" % l] = np.ascontiguousarray(inp["c_cmp_w2"][l].transpose(1, 0, 2))
        d["posT%d" % l] = np.ascontiguousarray(inp["c_cmp_pos"][l].transpose(2, 0, 1))
    d["fng"] = np.ascontiguousarray(inp["final_norm_g"][None, :])
    for k, v in CONSTS.items():
        d["k_" + k] = v
    return d


class Prog:
    def __init__(self, shapes, debug=()):
        self.nc = nc = bass.Bass("TRN2", target_bir_lowering=False)
        self.S = Sched(nc)
        self.debug = set(debug)
        self.inp = {}
        for name, (shape, dt) in shapes.items():
            self.inp[name] = nc.dram_tensor(name, list(shape), dt, kind="ExternalInput").ap()
        self.outs = {}
        self.arena = nc.alloc_sbuf_tensor("arena", [128, 207 * 1024], U8)
        self.top = 0
        self.psum = nc.alloc_psum_tensor("ps", [128, 4096], F32)

    def dram(self, name, shape, dt):
        t = self.nc.dram_tensor(name, list(shape), dt, kind="ExternalOutput").ap()
        self.outs[name] = t
        return t

    def sb(self, shape, dt):
        n = int(np.prod(shape[1:])) * _DTSZ[dt]
        a = self.arena[:, self.top:self.top + n].bitcast(dt)
        self.top += (n + 63) // 64 * 64
        self.maxtop = max(getattr(self, 'maxtop', 0), self.top)
        assert self.top <= 207 * 1024, "SBUF arena overflow %d" % self.top
        if len(shape) == 3:
            a = a.rearrange("p (a b) -> p a b", a=shape[1])
        elif len(shape) == 4:
            a = a.rearrange("p (a b c) -> p a b c", a=shape[1], b=shape[2])
        return a[: shape[0]] if shape[0] < 128 else a

    def bank(self, b, n=512, dt=F32, parts=128, off=0):
        a = self.psum[:, b * 512 + off: b * 512 + off + (n if dt == F32 else n // 2)]
        if dt != F32:
            a = a.bitcast(dt)
        return a[:parts] if parts < 128 else a

    def dma(self, out, in_, q="sp"):
        self.S.emit(q, lambda e: e.dma_start(out=out, in_=in_), w=[out], r=[in_], dma=True)

    def mm(self, out, lhsT, rhs, start=True, stop=True, xw=()):
        self.S.emit("pe", lambda e: e.matmul(out, lhsT, rhs, start=start, stop=stop), w=[out] + list(xw), r=[lhsT, rhs])

    def tr(self, out, in_, ident, xw=()):
        self.S.emit("pe", lambda e: e.transpose(out, in_, ident), w=[out] + list(xw), r=[in_, ident])

    def act(self, out, in_, func, bias=None, scale=None, accum=None, xr=()):
        kw = {}
        rr = [in_] + list(xr)
        ww = [out]
        if bias is not None:
            kw["bias"] = bias
            if not isinstance(bias, (int, float)):
                rr.append(bias)
        if scale is not None:
            kw["scale"] = scale
            if not isinstance(scale, (int, float)):
                rr.append(scale)
        if accum is not None:
            kw["accum_out"] = accum
            ww.append(accum)
        self.S.emit("act", lambda e: e.activation(out=out, in_=in_, func=func, **kw), w=ww, r=rr)

    def tt(self, eng, out, in0, in1, op):
        self.S.emit(eng, lambda e: e.tensor_tensor(out=out, in0=in0, in1=in1, op=op), w=[out], r=[in0, in1])

    def ts(self, eng, out, in0, s1, op0, s2=None, op1=None, xr=()):
        rr = [in0] + list(xr)
        for s in (s1, s2):
            if s is not None and not isinstance(s, (int, float)):
                rr.append(s)
        if op1 is None:
            self.S.emit(eng, lambda e: e.tensor_scalar(out=out, in0=in0, scalar1=s1, scalar2=None, op0=op0), w=[out], r=rr)
        else:
            self.S.emit(eng, lambda e: e.tensor_scalar(out=out, in0=in0, scalar1=s1, scalar2=s2, op0=op0, op1=op1), w=[out], r=rr)

    def stt(self, eng, out, in0, scalar, in1, op0, op1):
        rr = [in0, in1]
        if not isinstance(scalar, (int, float)):
            rr.append(scalar)
        self.S.emit(eng, lambda e: e.scalar_tensor_tensor(out=out, in0=in0, scalar=scalar, in1=in1, op0=op0, op1=op1), w=[out], r=rr)

    def cp(self, eng, out, in_, xr=()):
        if eng == "act":
            self.S.emit("act", lambda e: e.copy(out=out, in_=in_), w=[out], r=[in_] + list(xr))
        else:
            self.S.emit(eng, lambda e: e.tensor_copy(out=out, in_=in_), w=[out], r=[in_] + list(xr))

    def ms(self, eng, ap, val):
        self.S.emit(eng, lambda e: e.memset(ap, val), w=[ap])

    def red(self, eng, out, in_, op):
        self.S.emit(eng, lambda e: e.tensor_reduce(out=out, in_=in_, axis=AX.X, op=op), w=[out], r=[in_])

    def recip(self, out, in_):
        self.S.emit("dve", lambda e: e.reciprocal(out=out, in_=in_), w=[out], r=[in_])

    def recip_act(self, out, in_):
        self.act(out, in_, AF.Ln)
        self.act(out, out, AF.Exp, scale=-1.0)

    def dbg(self, name, ap, dt=F32):
        if name not in self.debug:
            return
        d = self.dram("dbg_" + name, list(ap.shape), dt)
        self.dma(d, ap)

    def load_consts(self):
        I = self.inp
        self.ident_f = self.sb([128, 128], F32)
        self.dma(self.ident_f, I["k_ident"])
        self.ident_b = self.sb([128, 128], BF16)
        self.cp("dve", self.ident_b, self.ident_f)
        self.ones_f = self.sb([128, 128], F32)
        self.ms("pool", self.ones_f, 1.0)
        self.ones_b = self.sb([128, 64], BF16)
        self.ms("pool", self.ones_b, 1.0)
        self.iota = self.sb([128, 128], F32)
        self.dma(self.iota, I["k_iota"])
        self.eps_ap = self.sb([128, 1], F32)
        self.ms("pool", self.eps_ap, 1.0e-6)
        ct = self.sb([128, 8], F32)
        self.dma(ct, I["cT"])
        self.cond = self.sb([128, 8], F32)
        self.act(self.cond, ct, AF.Silu)
        invf = self.sb([64, 1], F32)
        sgn = self.sb([64, 1], F32)
        self.dma(invf, I["k_invf"])
        self.dma(sgn, I["k_sgn"])
        mark = self.top
        self.cos64 = self.sb([64, SEQ], F32)
        self.sinS = self.sb([64, SEQ], F32)
        self.ropeS = self.dram("s_rope", [2, 64, SEQ], F32)
        posi = self.sb([64, SEQ], I32)
        self.dma(posi, I["pos"].partition_broadcast(64), q="pool")
        ang = self.sb([64, SEQ], F32)
        kf = self.sb([64, SEQ], F32)
        ki = self.sb([64, SEQ], I32)
        r = self.sb([64, SEQ], F32)
        self.cp("dve", ang, posi)
        self.ts("dve", ang, ang, invf[:, 0:1], ALU.mult)
        self.ts("dve", kf, ang, float(1.0 / (2 * np.pi)), ALU.mult)
        self.cp("dve", ki, kf)
        self.cp("dve", kf, ki)
        C1 = 6.28125
        C2 = float(2 * np.pi - 6.28125)
        self.stt("dve", r, kf, -C1, ang, ALU.mult, ALU.add)
        self.stt("dve", r, kf, -C2, r, ALU.mult, ALU.add)
        LIM = 3.1415925
        self.ts("dve", r, r, -LIM, ALU.max, LIM, ALU.min)
        self.act(self.sinS, r, AF.Sin)
        self.ts("dve", self.sinS, self.sinS, sgn[:, 0:1], ALU.mult)
        m = kf
        self.ts("dve", m, r, float(np.pi / 2), ALU.is_gt)
        self.stt("dve", r, m, float(-2 * np.pi), r, ALU.mult, ALU.add)
        self.ts("dve", r, r, float(np.pi / 2), ALU.add, -LIM, ALU.max)
        self.ts("dve", r, r, LIM, ALU.min)
        self.act(self.cos64, r, AF.Sin)
        self.dbg("cos64", self.cos64)
        self.dbg("sinS", self.sinS)
        self.dma(self.ropeS[0], self.cos64)
        self.dma(self.ropeS[1], self.sinS)
        self.top = mark

    def load_attn_consts(self):
        I = self.inp
        self.cmask = self.sb([128, 4, 512], BF16)
        self.dma(self.cmask, I["k_cmask"])
        self.wmask = self.sb([128, 4, 512], BF16)
        self.dma(self.wmask, I["k_wmask"])
        self.cos64 = self.sb([64, SEQ], F32)
        self.sinS = self.sb([64, SEQ], F32)
        self.dma(self.cos64, self.ropeS[0])
        self.dma(self.sinS, self.ropeS[1])

    def stage_mod(self, l):
        I = self.inp
        self.modT = self.sb([128, 48], F32)
        self.gt1 = self.sb([128, DM], F32)
        self.gt2 = self.sb([128, DM], F32)
        self.A1 = self.sb([128, 8], F32)
        self.A2 = self.sb([128, 8], F32)
        mark = self.top
        wst = [self.sb([128, 8, 512], F32) for _ in range(2)]
        modps = self.bank(0, 48)
        gps = {4: self.bank(1), 5: self.bank(2), 10: self.bank(3), 11: self.bank(4)}
        for g in range(12):
            w = wst[g % 2]
            self.dma(w, I["modw%d" % l][g], q="sp" if g % 2 == 0 else "pool")
            for j4 in range(4):
                j = g * 4 + j4
                for k in range(8):
                    self.mm(modps[:, j:j + 1], w[:, k, j4 * 128:(j4 + 1) * 128], self.cond[:, k:k + 1], start=(k == 0), stop=(k == 7))
            if g in gps:
                for k in range(8):
                    self.mm(gps[g], self.cond[:, k:k + 1].to_broadcast([128, 128]), w[:, k, :], start=(k == 0), stop=(k == 7))
        mb = self.sb([128, 48], F32)
        self.dma(mb, I["modbT%d" % l])
        self.tt("dve", self.modT, modps, mb, ALU.add)
        brow = self.sb([128, DM], F32)
        self.dma(brow, I["modbrow%d" % l][:, 2048:3072].partition_broadcast(128), q="pool")
        self.tt("dve", self.gt1[:, 0:512], gps[4], brow[:, 0:512], ALU.add)
        self.tt("dve", self.gt1[:, 512:1024], gps[5], brow[:, 512:1024], ALU.add)
        brow2 = self.sb([128, DM], F32)
        self.dma(brow2, I["modbrow%d" % l][:, 5120:6144].partition_broadcast(128), q="pool")
        self.tt("dve", self.gt2[:, 0:512], gps[10], brow2[:, 0:512], ALU.add)
        self.tt("dve", self.gt2[:, 512:1024], gps[11], brow2[:, 512:1024], ALU.add)
        g1 = self.sb([128, 8], F32)
        g2 = self.sb([128, 8], F32)
        self.dma(g1, I["gmix%d" % l])
        self.dma(g2, I["gffn%d" % l])
        self.stt("dve", self.A1, self.modT[:, 8:16], 1.0, g1, ALU.add, ALU.mult)
        self.stt("dve", self.A2, self.modT[:, 32:40], 1.0, g2, ALU.add, ALU.mult)
        self.dbg("modT%d" % l, self.modT)
        self.dbg("gt1_%d" % l, self.gt1)
        self.top = mark

    def norm_tile(self, xt, A, sh, dst, ps_bank):
        mark = self.top
        junk = self.sb([128, DM], BF16)
        ssq = self.sb([128, 1], F32)
        rstd = self.sb([128, 1], F32)
        xn = self.sb([128, DM], BF16)
        self.act(junk, xt, AF.Square, accum=ssq)
        self.act(rstd, ssq, AF.Sqrt, scale=float(1.0 / DM), bias=self.eps_ap[:, 0:1])
        self.recip(rstd, rstd)
        self.ts("dve", xn, xt, rstd[:, 0:1], ALU.mult)
        pT = self.bank(ps_bank, 1024, BF16)
        for k in range(8):
            self.tr(pT[:, k * 128:(k + 1) * 128], xn[:, k * 128:(k + 1) * 128], self.ident_b)
        tmp = self.sb([128, 8, 128], F32)
        self.tt("dve", tmp, pT.rearrange("p (k t) -> p k t", k=8), A[:, 0:8].unsqueeze(2).to_broadcast([128, 8, 128]), ALU.mult)
        self.tt("pool", dst, tmp, sh.unsqueeze(2).to_broadcast([128, 8, 128]), ALU.add)
        self.top = mark

    def stage_h(self, l, xsrc):
        I = self.inp
        self.hT = self.sb([128, 8, SEQ], BF16)
        mark = self.top
        xts = [self.sb([128, DM], F32) for _ in range(2)]
        for tt in range(16):
            xt = xts[tt % 2]
            self.dma(xt, xsrc[tt * 128:(tt + 1) * 128, :], q="sp" if tt % 2 == 0 else "pool")
            self.norm_tile(xt, self.A1, self.modT[:, 0:8], self.hT[:, :, tt * 128:(tt + 1) * 128], 5 + (tt % 2))
        self.top = mark
        if ("hT%d" % l) in self.debug:
            d = self.dram("dbg_hT%d" % l, [128, 8, SEQ], BF16)
            self.dma(d, self.hT)

    def load_blk(self, l, name, dst=None, q="pool"):
        if dst is None:
            dst = self.sb([128, 8, 128], BF16)
        self.dma(dst, self.inp["win%d" % l][BLK[name]], q=q)
        return dst

    def proj(self, ps, wb, tg, n=512):
        for k in range(8):
            self.mm(ps, wb[:, k, :], self.hT[:, k, tg * 512: tg * 512 + n], start=(k == 0), stop=(k == 7))

    def stage_mixA(self, l):
        I = self.inp
        mark = self.top
        acw = self.sb([128, 4, 3], F32)
        self.dma(acw, I["acw%d" % l])
        wc = [self.sb([128, 8, 128], BF16) for _ in range(2)]
        wx = [self.sb([128, 8, 128], BF16) for _ in range(2)]
        wb = [self.sb([128, 8, 128], BF16) for _ in range(2)]
        zbuf = self.sb([128, 2 + SEQ], F32)
        y = self.sb([128, SEQ], F32)
        am = [self.sb([128, SEQ], BF16) for _ in range(2)]
        tmpc = [self.sb([128, 512], F32) for _ in range(2)]
        self.ms("pool", zbuf[:, 0:2], 0.0)
        for ch in range(4):
            b = ch % 2
            self.load_blk(l, "a_c%d" % ch, wc[b])
            self.load_blk(l, "a_x%d" % ch, wx[b])
            self.load_blk(l, "a_b%d" % ch, wb[b])
            for tg in range(4):
                pc = self.bank(0 + 2 * (tg % 2))
                px = self.bank(1 + 2 * (tg % 2))
                self.proj(pc, wc[b], tg)
                self.proj(px, wx[b], tg)
                self.cp("act", tmpc[tg % 2], pc)
                self.tt("dve", zbuf[:, 2 + tg * 512: 2 + (tg + 1) * 512], tmpc[tg % 2], px, ALU.mult)
            self.ts("dve", y, zbuf[:, 0:SEQ], acw[:, ch, 0:1], ALU.mult)
            self.stt("dve", y, zbuf[:, 1:SEQ + 1], acw[:, ch, 1:2], y, ALU.mult, ALU.add)
            self.stt("dve", y, zbuf[:, 2:SEQ + 2], acw[:, ch, 2:3], y, ALU.mult, ALU.add)
            for tg in range(4):
                pb = self.bank(4 + (tg % 2))
                self.proj(pb, wb[b], tg)
                self.tt("dve", am[b][:, tg * 512:(tg + 1) * 512], y[:, tg * 512:(tg + 1) * 512], pb, ALU.mult)
            self.dma(self.amixT[:, ch, :], am[b])
        self.top = mark

    def stage_mixB(self, l):
        I = self.inp
        mark = self.top
        bcw = self.sb([128, 4, 31], F32)
        bcb = self.sb([128, 4], F32)
        blg = self.sb([128, 4], F32)
        blb = self.sb([128, 4], F32)
        self.dma(bcw, I["bcw%d" % l])
        self.dma(bcb, I["bcb%d" % l])
        self.dma(blg, I["blg%d" % l])
        self.dma(blb, I["blb%d" % l])
        wa = [self.sb([128, 8, 128], BF16) for _ in range(2)]
        wg = [self.sb([128, 8, 128], BF16) for _ in range(2)]
        ubuf = [self.sb([128, 30 + SEQ], F32) for _ in range(2)]
        yc = self.sb([128, 4, SEQ], F32)
        sg = [self.sb([128, 512], F32) for _ in range(2)]
        for ch in range(4):
            b = ch % 2
            eng = "dve"
            self.load_blk(l, "b_a%d" % ch, wa[b])
            self.load_blk(l, "b_g%d" % ch, wg[b])
            self.ms("pool", ubuf[b][:, 0:30], 0.0)
            for tg in range(4):
                pa = self.bank(0 + 2 * (tg % 2))
                pg = self.bank(1 + 2 * (tg % 2))
                self.proj(pa, wa[b], tg)
                self.proj(pg, wg[b], tg)
                self.act(sg[tg % 2], pg, AF.Sigmoid)
                self.tt("dve", ubuf[b][:, 30 + tg * 512: 30 + (tg + 1) * 512], sg[tg % 2], pa, ALU.mult)
            yv = yc[:, ch, :]
            self.ts(eng, yv, ubuf[b][:, 0:SEQ], bcw[:, ch, 0:1], ALU.mult, bcb[:, ch:ch + 1], ALU.add)
            for k in range(1, 31):
                self.stt(eng, yv, ubuf[b][:, k:k + SEQ], bcw[:, ch, k:k + 1], yv, ALU.mult, ALU.add)
        self.dbg("bconv%d" % l, yc.rearrange("p a b -> p (a b)"))
        bm = self.sb([128, 4, SEQ], BF16)
        sq = [self.sb([128, 512], F32) for _ in range(2)]
        mean = self.sb([128, 512], F32)
        rstd = self.sb([128, 512], F32)
        t1 = [self.sb([128, 512], F32) for _ in range(2)]
        for tg in range(4):
            sl = slice(tg * 512, (tg + 1) * 512)
            ps_s = self.bank(4)
            ps_q = self.bank(5)
            for ch in range(4):
                self.mm(ps_s, self.ones_f, yc[:, ch, sl], start=(ch == 0), stop=(ch == 3))
            for ch in range(4):
                self.act(sq[ch % 2], yc[:, ch, sl], AF.Square)
                self.mm(ps_q, self.ones_f, sq[ch % 2], start=(ch == 0), stop=(ch == 3))
            self.ts("dve", mean, ps_s, float(1.0 / 512), ALU.mult)
            self.tt("dve", rstd, mean, mean, ALU.mult)
            self.stt("dve", rstd, ps_q, float(1.0 / 512), rstd, ALU.mult, ALU.subtract)
            self.act(rstd, rstd, AF.Sqrt, bias=self.eps_ap[:, 0:1])
            self.recip(rstd, rstd)
            for ch in range(4):
                t = t1[ch % 2]
                self.tt("dve", t, yc[:, ch, sl], mean, ALU.subtract)
                self.tt("pool", t, t, rstd, ALU.mult)
                self.act(bm[:, ch, sl], t, AF.Silu, bias=blb[:, ch:ch + 1], scale=blg[:, ch:ch + 1])
        self.dma(self.bmixT, bm)
        self.top = mark


def stage_peer2(P, l, xsrc, xdst, final):
    I = P.inp
    mark = P.top
    NG = 8
    if "peer_1group" in P.debug:
        NG = 1
    skT = P.sb([128, 16, 128], F32)
    P.dma(skT, I["skT%d" % l])
    W_sb = P.sb([128, 256, 128], BF16)
    h2T = [P.sb([128, 8, 256], BF16) for _ in range(2)]
    I3T = [P.sb([128, 3, 256], F32) for _ in range(2)]
    xt2 = [P.sb([128, DM], F32) for _ in range(2)]
    uTb = [P.sb([128, 2, 8, 128], BF16) for _ in range(2)]
    vbb = [P.sb([128, 2, DM], BF16) for _ in range(2)]
    utv = P.utS.rearrange("c p k i -> p c k i")
    vbv2 = P.vbS.rearrange("c i d -> i c d")
    Gb = [P.sb([128, 256], BF16) for _ in range(2)]
    Wa = [P.sb([128, 256], BF16) for _ in range(3)]
    etmp = [P.sb([128, 512], F32) for _ in range(2)]
    if final:
        fng = P.sb([128, DM], F32)
        P.dma(fng, I["fng"].partition_broadcast(128), q="pool")
        ot = [P.sb([128, DM], F32) for _ in range(2)]
        fjunk = P.sb([128, DM], BF16)
        fssq = P.sb([128, 1], F32)
        frstd = P.sb([128, 1], F32)
    mT = P.top
    G6 = P.bank(6)
    G7 = P.bank(7)
    sc_sb = P.sb([128, 2, 16, 128], F32)
    mT2 = P.top
    xtp = [P.sb([128, DM], F32) for _ in range(2)]
    wqb = [P.sb([128, 8, 128], BF16) for _ in range(2)]
    qf = [P.sb([128, 256], F32) for _ in range(2)]
    sq = [P.sb([128, 256], F32) for _ in range(2)]
    rs = [P.sb([128, 256], F32) for _ in range(2)]
    qn = [P.sb([128, 256], F32) for _ in range(2)]
    norm_base = P.top
    P.top = mT2
    V = P.sb([128, 16, 16], F32)
    IDX = P.sb([128, 16, 16], U32)
    IDXf = P.sb([128, 16, 16], F32)
    tmpa = [P.sb([128, 128], F32) for _ in range(4)]
    cand = P.sb([128, 8, 256], F32)
    tmpc = [P.sb([128, 256], F32) for _ in range(4)]
    TOP = P.sb([128, 8, 16], F32)
    CI = P.sb([128, 8, 16], U32)
    AKu = P.sb([128, 8, 16], U32)
    BKu = P.sb([128, 8, 16], U32)
    AK = P.sb([128, 8, 16], F32)
    BK = P.sb([128, 8, 16], F32)
    oh = P.sb([128, 128, 16], F32)
    I1S = P.sb([128, 128], F32)
    I2S = P.sb([128, 128], F32)
    dsm = P.sb([128, 8, 16], F32)
    ssum = P.sb([128, 8], F32)
    Gm = P.sb([128, 8, 16], F32)
    pre_end = max(P.top, norm_base + 9 * 1024)
    iota16 = P.iota[:, 0:16].unsqueeze(1).unsqueeze(1).to_broadcast([128, 8, 16, 16])
    P.top = mT
    A = [P.sb([128, 16, 128], BF16) for _ in range(2)]
    B = [P.sb([128, 16, 128], BF16) for _ in range(2)]
    P.top = max(P.top, pre_end)
    P.maxtop = max(P.maxtop, P.top)
    assert P.top <= 207 * 1024, P.top

    def pre_slices(g):
        par = g % 2
        t0 = g * 256
        out = []

        def norm(t2):
            def f():
                old = P.top
                P.top = norm_base
                P.dma(xtp[t2], xsrc[t0 + t2 * 128: t0 + (t2 + 1) * 128, :])
                P.norm_tile(xtp[t2], P.A2, P.modT[:, 24:32], h2T[par][:, :, t2 * 128:(t2 + 1) * 128], 6 + t2)
                P.top = old
            return f

        out.append(norm(0))
        out.append(norm(1))

        def q_s1(j):
            b = j % 2
            P.dma(wqb[b], I["wq%d" % l][:, :, j * 128:(j + 1) * 128], q="pool")
            q_ps = P.bank(6)[:, 0:256]
            for k in range(8):
                P.mm(q_ps, wqb[b][:, k, :], h2T[par][:, k, :], start=(k == 0), stop=(k == 7), xw=[G6])
            P.cp("act", qf[b], q_ps, xr=[G6])
            P.act(sq[b], q_ps, AF.Square, xr=[G6])

        def q_s2(j):
            b = j % 2
            s_ps = P.bank(7)[:, 0:256]
            P.mm(s_ps, P.ones_f, sq[b], xw=[G7])
            P.ts("dve", rs[b], s_ps, float(1.0 / 128), ALU.mult, 1.0e-6, ALU.add, xr=[G7])
            P.act(rs[b], rs[b], AF.Sqrt)
            P.recip(rs[b], rs[b])
            P.tt("dve", qn[b], qf[b], rs[b], ALU.mult)
            c_ps = P.bank(7)[:, 256:512]
            for t2 in range(2):
                P.mm(c_ps[:, t2 * 128:(t2 + 1) * 128], qn[b][:, t2 * 128:(t2 + 1) * 128], skT[:, j, :], xw=[G7])
            P.cp("dve", sc_sb[:, :, j, :], c_ps.rearrange("p (a n) -> p a n", a=2), xr=[G7])

        out.append(lambda: q_s1(0))
        for j in range(16):
            def f(j=j):
                if j + 1 < 16:
                    q_s1(j + 1)
                q_s2(j)
            out.append(f)

        outA = out
        out = []
        for t2 in range(2):
            for j0 in range(0, 16, 4):
                out.append(lambda t2=t2, j0=j0: _top16_multi(P, [(sc_sb[:, t2, j, :], V[:, j, :], IDX[:, j, :], tmpa[j % 4]) for j in range(j0, j0 + 4)]))

            def f1(t2=t2):
                P.cp("dve", IDXf, IDX)
                V4 = V.rearrange("p (h two) a -> p h two a", two=2)
                P.tt("dve", cand.rearrange("p h (a b) -> p h a b", a=16),
                     V4[:, :, 0, :].unsqueeze(3).to_broadcast([128, 8, 16, 16]),
                     V4[:, :, 1, :].unsqueeze(2).to_broadcast([128, 8, 16, 16]), ALU.add)
            out.append(f1)
            for h0 in range(0, 8, 4):
                out.append(lambda h0=h0: _top16_multi(P, [(cand[:, h, :], TOP[:, h, :], CI[:, h, :], tmpc[h % 4]) for h in range(h0, h0 + 4)]))

            def f2(t2=t2):
                P.ts("dve", AKu, CI, 4, ALU.logical_shift_right)
                P.ts("dve", BKu, CI, 15, ALU.bitwise_and)
                P.cp("dve", AK, AKu)
                P.cp("dve", BK, BKu)
                IDX4 = IDXf.rearrange("p (h two) a -> p h two a", two=2)
                oh4 = oh.rearrange("p (h k) a -> p h k a", h=8)
                for XK, pidx, OUT in ((AK, 0, I1S), (BK, 1, I2S)):
                    P.tt("dve", oh4, iota16, XK.unsqueeze(3).to_broadcast([128, 8, 16, 16]), ALU.is_equal)
                    P.tt("dve", oh4, oh4, IDX4[:, :, pidx, :].unsqueeze(2).to_broadcast([128, 8, 16, 16]), ALU.mult)
                    P.red("dve", OUT, oh, ALU.add)
            out.append(f2)

            def f3(t2=t2):
                P.tt("dve", dsm, TOP, TOP[:, :, 0:1].to_broadcast([128, 8, 16]), ALU.subtract)
                P.act(dsm, dsm, AF.Exp)
                P.red("dve", ssum, dsm, ALU.add)
                P.recip(ssum, ssum)
                P.tt("dve", Gm, dsm, ssum.unsqueeze(2).to_broadcast([128, 8, 16]), ALU.mult)
                tr_ps = P.bank(6)[:, 0:384]
                P.tr(tr_ps[:, 0:128], I1S, P.ident_f, xw=[G6])
                P.tr(tr_ps[:, 128:256], I2S, P.ident_f, xw=[G6])
                P.tr(tr_ps[:, 256:384], Gm.rearrange("p h k -> p (h k)"), P.ident_f, xw=[G6])
                P.cp("act", I3T[par][:, :, t2 * 128:(t2 + 1) * 128], tr_ps.rearrange("p (a t) -> p a t", a=3), xr=[G6])
            out.append(f3)
        return outA, out

    def w_build(g):
        par = g % 2

        def ab_build(sbi):
            ab = sbi % 2
            tsl = slice(sbi * 16, sbi * 16 + 16)
            P.tt("dve", B[ab], P.iota.unsqueeze(1).to_broadcast([128, 16, 128]),
                 I3T[par][:, 1, tsl].unsqueeze(2).to_broadcast([128, 16, 128]), ALU.is_equal)
            for tl in range(16):
                t = sbi * 16 + tl
                P.ts("dve", A[ab][:, tl, :], P.iota, I3T[par][:, 0, t:t + 1], ALU.is_equal, I3T[par][:, 2, t:t + 1], ALU.mult)

        ab_build(0)
        for sbi in range(16):
            ab = sbi % 2
            for q4 in range(4):
                w_ps = P.bank(q4)
                for u in range(4):
                    tl = q4 * 4 + u
                    P.mm(w_ps[:, u * 128:(u + 1) * 128], B[ab][:, tl, :], A[ab][:, tl, :])
            if sbi + 1 < 16:
                ab_build(sbi + 1)
            for q4 in range(4):
                w_ps = P.bank(q4)
                tb = sbi * 16 + q4 * 4
                P.cp("act" if q4 % 2 == 0 else "dve", W_sb[:, tb:tb + 4, :], w_ps.rearrange("p (t c) -> p t c", t=4))

    def main_loop(g, slices):
        par = g % 2

        def s_mm(c):
            if c % 2 == 0:
                P.dma(uTb[(c // 2) % 2], utv[:, c:c + 2])
                P.dma(vbb[(c // 2) % 2], vbv2[:, c:c + 2, :])
            ub_ = uTb[(c // 2) % 2][:, c % 2]
            S_ps = P.bank(4 + c % 2)[:, 0:256]
            for k in range(8):
                P.mm(S_ps, ub_[:, k, :], h2T[par][:, k, :], start=(k == 0), stop=(k == 7))

        ns = len(slices)
        done = 0
        s_mm(0)
        for c in range(128):
            if c + 1 < 128:
                s_mm(c + 1)
            vb_ = vbb[(c // 2) % 2][:, c % 2]
            S_ps = P.bank(4 + c % 2)[:, 0:256]
            P.act(Gb[c % 2], S_ps, AF.Gelu)
            wa = Wa[c % 3]
            P.tt("dve" if c % 2 == 0 else "pool", wa, Gb[c % 2], W_sb[:, :, c], ALU.mult)
            for t2 in range(2):
                for half in range(2):
                    P.mm(P.bank(t2 * 2 + half), wa[:, t2 * 128:(t2 + 1) * 128], vb_[:, half * 512:(half + 1) * 512],
                         start=(c == 0), stop=(c == 127))
            want = min(ns, ((c + 1) * ns + 111) // 112)
            while done < want:
                slices[done]()
                done += 1
        while done < ns:
            slices[done]()
            done += 1

    def epilogue(g):
        t0 = g * 256
        for t2 in range(2):
            x_ = xt2[t2]
            r0 = t0 + t2 * 128
            P.dma(x_, xsrc[r0:r0 + 128, :])
            for half in range(2):
                hs = slice(half * 512, (half + 1) * 512)
                e_ = etmp[half]
                P.tt("dve", e_, P.gt2[:, hs], P.bank(t2 * 2 + half), ALU.mult)
                P.tt("pool", x_[:, hs], x_[:, hs], e_, ALU.add)
            if not final:
                P.dma(xdst[r0:r0 + 128, :], x_)
            else:
                P.act(fjunk, x_, AF.Square, accum=fssq)
                P.act(frstd, fssq, AF.Sqrt, scale=float(1.0 / DM), bias=P.eps_ap[:, 0:1])
                P.recip(frstd, frstd)
                o_ = ot[t2]
                P.ts("dve", o_, x_, frstd[:, 0:1], ALU.mult)
                P.tt("pool", o_, o_, fng, ALU.mult)
                P.dma(xdst[r0:r0 + 128, :], o_)

    pa, pb = pre_slices(0)
    for f in pa + pb:
        f()
    for g in range(NG):
        w_build(g)
        pb = []
        if g + 1 < NG:
            pa, pb = pre_slices(g + 1)
            for f in pa:
                f()
        main_loop(g, pb)
        epilogue(g)
    P.top = mark


def input_shapes(shared, core):
    shapes = {}
    for k, v in list(shared.items()) + list(core.items()):
        dt = {np.dtype(np.float32): F32, np.dtype(np.int32): I32, np.dtype(NPBF): BF16}[v.dtype]
        shapes[k] = (v.shape, dt)
    return shapes


def build(shapes, upto="all", debug=()):
    P = Prog(shapes, debug)
    I = P.inp
    P.load_consts()
    P.amixT = P.dram("s_amixT", [128, 4, SEQ], BF16)
    P.bmixT = P.dram("s_bmixT", [128, 4, SEQ], BF16)
    P.ocT = P.dram("s_ocT", [8, 64, SEQ], BF16)
    P.odT = P.dram("s_odT", [8, 64, SEQ], BF16)
    if upto in ("peer", "peer_only", "all"):
        P.utS = P.dram("s_ut", [128, 128, 8, 128], BF16)
        P.vbS = P.dram("s_vb", [128, 128, DM], BF16)
    xsrc = I["xin"]
    for l in range(DEPTH):
        base = P.top
        P.stage_mod(l)
        xmid = P.dram("s_xmid%d" % l, [SEQ, DM], F32)
        if upto == "peer_only":
            stage_peer_prep(P, l)
            xo = P.dram("s_xout%d" % l, [SEQ, DM], F32)
            stage_peer2(P, l, I["xmid_in"], xo, False)
            break
        hmark = P.top
        P.stage_h(l, xsrc)
        if upto not in ("nsa_only", "moba_only"):
            P.stage_mixA(l)
        if upto == "mixA":
            break
        if upto not in ("nsa_only", "moba_only"):
            P.stage_mixB(l)
        if upto == "mixB":
            break
        if upto != "moba_only":
            stage_nsa(P, l)
        if upto in ("nsa", "nsa_only"):
            break
        stage_moba(P, l)
        if upto in ("moba", "moba_only"):
            break
        stage_merge(P, l, xsrc, xmid)
        if upto == "merge":
            break
        P.top = hmark
        stage_peer_prep(P, l)
        if l == DEPTH - 1:
            xo = P.dram("out", [SEQ, DM], F32)
        else:
            xo = P.dram("s_xout%d" % l, [SEQ, DM], F32)
        stage_peer2(P, l, xmid, xo, l == DEPTH - 1)
        if upto == "peer":
            break
        xsrc = xo
        P.top = base
    P.stats = P.S.finalize()
    return P


def core_inputs(inp, b):
    return dict(
        xin=np.ascontiguousarray(inp["x"][b]),
        cT=np.ascontiguousarray(inp["c"][b].reshape(8, 128).T),
        pos=np.ascontiguousarray(inp["positions"][b][None, :].astype(np.int32)),
    )


def _rope_evac(P, p1, p2, rows, dst, tg, i):
    sl = slice(tg * 512, (tg + 1) * 512)
    t1 = P.rt1[i % 2]
    t2 = P.rt2[i % 2]
    P.tt("dve", t1, P.cos64[:, sl], p1[rows], ALU.mult)
    P.tt("dve", t2, P.sinS[:, sl], p2[rows], ALU.mult)
    P.tt("pool", dst, t1, t2, ALU.add)


SBANKS = (0, 1, 6)


def _run_tiles(P, tiles, look=2):
    n = len(tiles)
    slot = [None] * n

    def qk(i):
        j = P.tile_ctr
        P.tile_ctr += 1
        slot[i] = j
        P.mm(P.bank(SBANKS[j % 3]), tiles[i]["kT"], tiles[i]["qT"])

    for i in range(min(look, n)):
        qk(i)
    for i in range(n):
        if i + look < n:
            qk(i + look)
        t = tiles[i]
        j = slot[i]
        s_ps = P.bank(SBANKS[j % 3])
        E = P.Ebuf[j % 3]
        P.act(E, s_ps, AF.Exp, scale=0.125)
        if t["mask"] is not None:
            P.tt("dve" if j % 2 == 0 else "pool", E, E, t["mask"], ALU.mult)
        P.mm(t["num"], t["vT"], E, start=t["first"], stop=t["last"])
        P.mm(t["den"], P.ones_b, E, start=t["first"], stop=t["last"])
        if t["post"] is not None:
            t["post"]()


def _grep(P, br, h, tg, i):
    g_ps = P.bank(7)[0:64]
    P.mm(g_ps, P.selall[:, br * 8 + h, :], P.GS[:, tg * 512:(tg + 1) * 512])
    gr = P.grbuf[i % 2]
    P.cp("act", gr, g_ps)
    return gr


def stage_nsa(P, l):
    I = P.inp
    mark = P.top
    P.load_attn_consts()
    QA = P.sb([128, 8, SEQ], BF16)
    KC = P.sb([64, 2, SEQ + 16], BF16)
    VCt = P.sb([64, 2, SEQ + 16], BF16)
    KS = P.sb([128, 2, SEQ], BF16)
    KW = P.sb([64, 2, SEQ], BF16)
    VS = P.sb([128, 16, 128], BF16)
    VW = P.sb([128, 16, 128], BF16)
    P.GS = P.sb([32, SEQ], BF16)
    KCMP = P.sb([64, 2, 128], BF16)
    VCMP = P.sb([128, 2, 64], BF16)
    mA = P.top
    P.rt1 = [P.sb([64, 512], F32) for _ in range(2)]
    P.rt2 = [P.sb([64, 512], F32) for _ in range(2)]
    wbl = [P.sb([128, 8, 128], BF16) for _ in range(4)]
    cnt = 0
    for j in range(4):
        w1 = P.load_blk(l, "c_q%d" % j, wbl[(2 * j) % 4])
        w2 = P.load_blk(l, "c_q_sw%d" % j, wbl[(2 * j + 1) % 4])
        for tg in range(4):
            p1 = P.bank(0 + 2 * (cnt % 2))
            p2 = P.bank(1 + 2 * (cnt % 2))
            P.proj(p1, w1, tg)
            P.proj(p2, w2, tg)
            sl = slice(tg * 512, (tg + 1) * 512)
            _rope_evac(P, p1, p2, slice(0, 64), QA[0:64, 2 * j, sl], tg, cnt)
            _rope_evac(P, p1, p2, slice(64, 128), QA[0:64, 2 * j + 1, sl], tg, cnt + 1)
            cnt += 1
    for nm, dstT in (("c_kc", KC), ("c_ks", KS), ("c_kw", KW)):
        w1 = P.load_blk(l, nm, wbl[0])
        w2 = P.load_blk(l, nm + "_sw", wbl[1])
        for tg in range(4):
            p1 = P.bank(0 + 2 * (cnt % 2))
            p2 = P.bank(1 + 2 * (cnt % 2))
            P.proj(p1, w1, tg)
            P.proj(p2, w2, tg)
            sl = slice(tg * 512, (tg + 1) * 512)
            _rope_evac(P, p1, p2, slice(0, 64), dstT[0:64, 0, sl], tg, cnt)
            _rope_evac(P, p1, p2, slice(64, 128), dstT[0:64, 1, sl], tg, cnt + 1)
            cnt += 1
    w1 = P.load_blk(l, "c_vc", wbl[2])
    for tg in range(4):
        p1 = P.bank(4 + (tg % 2))
        P.proj(p1, w1, tg)
        sl = slice(tg * 512, (tg + 1) * 512)
        P.cp("act", VCt[:, 0, sl], p1[0:64])
        P.cp("dve", VCt[:, 1, sl], p1[64:128])
    w1 = P.load_blk(l, "c_gate", wbl[3])
    for tg in range(4):
        p1 = P.bank(6 + (tg % 2))
        P.proj(p1, w1, tg)
        P.act(P.GS[:, tg * 512:(tg + 1) * 512], p1[0:32], AF.Sigmoid)
    for nm, dstV in (("c_vs", VS), ("c_vw", VW)):
        w1 = P.load_blk(l, nm, wbl[0] if nm == "c_vs" else wbl[1])
        for t16 in range(16):
            ps = P.bank(4 + (t16 % 2))[:, 0:128]
            for k in range(8):
                P.mm(ps, P.hT[:, k, t16 * 128:(t16 + 1) * 128], w1[:, k, :], start=(k == 0), stop=(k == 7))
            P.cp("act" if t16 % 2 == 0 else "dve", dstV[:, t16, :], ps)
    P.ms("pool", KS[64:128], 0.0)
    P.ms("pool", QA[64:128], 0.0)
    for g in range(2):
        P.dma(KS[64:96, g, :], I["k_slcaug"])
    if "stop_proj" in P.debug:
        P.top = mark
        return
    P.top = mA
    w1c = P.sb([64, 2, 32, 128], BF16)
    P.dma(w1c, I["w1r%d" % l], q="pool")
    w2c = P.sb([128, 2, 64], BF16)
    P.dma(w2c, I["w2r%d" % l], q="pool")
    posf = P.sb([64, 2, 32], F32)
    P.dma(posf, I["posT%d" % l])
    hid = [P.sb([128, 128], BF16) for _ in range(2)]
    srcA = [P.sb([64, SEQ], BF16) for _ in range(2)]
    srcB = [P.sb([64, SEQ], BF16) for _ in range(2)]
    for g in range(2):
        P.ms("pool", KC[:, g, SEQ:SEQ + 16], 0.0)
        P.ms("pool", VCt[:, g, SEQ:SEQ + 16], 0.0)
    ci = 0
    for kv in range(2):
        SRC = KC if kv == 0 else VCt
        for g in range(2):
            sa = srcA[ci % 2]
            sbb = srcB[ci % 2]
            ci += 1
            P.tt("dve", sa.rearrange("p (n s) -> p n s", s=16), SRC[:, g, 0:SEQ].rearrange("p (n s) -> p n s", s=16),
                 posf[:, kv, 0:16].unsqueeze(1).to_broadcast([64, 128, 16]), ALU.add)
            P.tt("dve", sbb.rearrange("p (n s) -> p n s", s=16), SRC[:, g, 16:SEQ + 16].rearrange("p (n s) -> p n s", s=16),
                 posf[:, kv, 16:32].unsqueeze(1).to_broadcast([64, 128, 16]), ALU.add)
            sa3 = sa.rearrange("p (n s) -> p n s", s=16)
            sb3 = sbb.rearrange("p (n s) -> p n s", s=16)
            h_ps = P.bank(7)[:, 0:128]
            for li in range(32):
                rhs = sa3[:, :, li] if li < 16 else sb3[:, :, li - 16]
                P.mm(h_ps, w1c[:, kv, li, :], rhs, start=(li == 0), stop=(li == 31))
            hd = hid[g]
            P.act(hd, h_ps, AF.Gelu)
            if kv == 0:
                kc_ps = P.bank(6)[0:64, 128:256]
                P.mm(kc_ps, w2c[:, 0, :], hd)
                P.cp("act", KCMP[:, g, :], kc_ps)
            else:
                vc_ps = P.bank(6)[:, 256:320]
                P.mm(vc_ps, hd, w2c[:, 1, :])
                P.cp("act", VCMP[:, g, :], vc_ps)
    if "stop_cmp" in P.debug:
        P.top = mark
        return
    P.top = mA
    cmpmask = P.sb([128, SEQ], BF16)
    P.dma(cmpmask, I["k_cmpmask"])
    overlap = P.sb([128, 32], F32)
    P.dma(overlap, I["k_overlap"])
    slcadd = P.sb([128, 16, 32], F32)
    P.dma(slcadd, I["k_slcadd"])
    P.selall = P.sb([32, 24, 64], BF16)
    P.dma(P.selall, I["k_selall"])
    SBm = P.sb([128, 4, 2, 128], BF16)
    P.ms("pool", SBm, 0.0)
    OC = P.sb([64, 8, 512], F32)
    P.Ebuf = [P.sb([128, 512], BF16) for _ in range(3)]
    P.grbuf = [P.sb([64, 512], F32) for _ in range(2)]
    Ef = [P.sb([128, 512], F32) for _ in range(2)]
    rdf = [P.sb([128, 512], F32) for _ in range(2)]
    Pn = [P.sb([128, 512], F32) for _ in range(2)]
    Pb = [P.sb([128, 512], BF16) for _ in range(2)]
    vsel = [P.sb([128, 32], F32) for _ in range(2)]
    psum_g = P.sb([128, 512], F32)
    vtmp = [P.sb([128, 32], F32) for _ in range(2)]
    m8a = [P.sb([128, 8], F32) for _ in range(2)]
    m8b = [P.sb([128, 8], F32) for _ in range(2)]
    rd64 = [P.sb([64, 512], F32) for _ in range(2)]
    ctmp = [P.sb([64, 512], F32) for _ in range(2)]
    ocb = [P.sb([64, 512], BF16) for _ in range(2)]
    dbg_br = {}
    for nm in ("cmp", "slc", "win"):
        if ("ob_%s" % nm) in P.debug:
            dbg_br[nm] = P.dram("dbg_ob_%s" % nm, [8, 64, SEQ], F32)
    P.tile_ctr = 0
    gi = 0
    ei = 0
    for tg in range(4):
        sl = slice(tg * 512, (tg + 1) * 512)
        imp4 = P.bank(6)[:, 0:256].rearrange("p (a b c) -> p a b c", a=4, b=2)
        for h in range(8):
            g, r = h // 4, h % 4
            s_ps = P.bank(h % 2)
            P.mm(s_ps, KCMP[:, g, :], QA[0:64, h, sl])
            E = Ef[h % 2]
            P.act(E, s_ps, AF.Exp, scale=0.125)
            P.tt("dve", E, E, cmpmask[:, sl], ALU.mult)
            d_ps = P.bank(2 + h % 2)
            P.mm(d_ps, P.ones_f, E)
            rd = rdf[h % 2]
            P.ts("dve", rd, d_ps, 1.0e-18, ALU.max)
            P.recip_act(rd, rd)
            pn = Pn[h % 2]
            P.tt("dve", pn, E, rd, ALU.mult)
            pb = Pb[h % 2]
            P.cp("pool", pb, pn)
            o_ps = P.bank(4 + h % 2)[0:64]
            P.mm(o_ps, VCMP[:, g, :], pb)
            if r == 0:
                P.cp("pool", psum_g, pn)
            else:
                P.tt("pool", psum_g, psum_g, pn, ALU.add)
            if r == 3:
                for t4 in range(4):
                    P.mm(imp4[:, t4, g, :], psum_g[:, t4 * 128:(t4 + 1) * 128], overlap)
            gr = _grep(P, 0, h, tg, gi)
            gi += 1
            P.tt("dve", OC[:, h, :], gr, o_ps, ALU.mult)
            if "cmp" in dbg_br:
                P.cp("dve", ctmp[0], o_ps)
                P.dma(dbg_br["cmp"][h, :, sl], ctmp[0])
        if "imp" in P.debug and tg == 0:
            impd = P.dram("dbg_imp", [128, 256], F32)
            impsb = P.sb([128, 256], F32)
            P.cp("dve", impsb, P.bank(6)[:, 0:256])
            P.dma(impd, impsb)
        if "stop_c1" in P.debug:
            break
        for t4 in range(4):
            for g in range(2):
                i2 = (t4 * 2 + g) % 2
                v = vsel[i2]
                P.tt("dve", v, slcadd[:, tg * 4 + t4, :], imp4[:, t4, g, :], ALU.add)
                P.S.emit("dve", lambda e, o=m8a[i2], i=v: e.max(out=o, in_=i), w=[m8a[i2]], r=[v])
                P.S.emit("dve", lambda e, o=vtmp[i2], a=m8a[i2], b=v: e.match_replace(out=o, in_to_replace=a, in_values=b, imm_value=-3.0e38), w=[vtmp[i2]], r=[m8a[i2], v])
                P.S.emit("dve", lambda e, o=m8b[i2], i=vtmp[i2]: e.max(out=o, in_=i), w=[m8b[i2]], r=[vtmp[i2]])
                P.ts("dve", SBm[:, t4, g, 64:96], v, m8b[i2][:, 7:8], ALU.is_ge, 1.0, ALU.subtract)
                bt = P.bank(0 + g)[:, t4 * 128:(t4 + 1) * 128]
                P.mm(bt, SBm[:, t4, g, :], P.ident_b)
        for g in range(2):
            for r in range(4):
                P.cp("act" if g == 0 else "dve", QA[64:96, 4 * g + r, sl], P.bank(0 + g)[64:96, :])
        if "stop_s1" in P.debug:
            break
        tiles = []
        for h in range(8):
            g = h // 4
            for bi, nm in ((1, "slc"), (2, "win")):
                num = P.bank(2 + 2 * (ei % 2))[0:64]
                den = P.bank(3 + 2 * (ei % 2))[0:64]
                if nm == "slc":
                    kts = list(range(0, 4 * tg + 4))
                else:
                    kts = list(range(max(0, 4 * tg - 4), 4 * tg + 4))

                def post(h=h, bi=bi, nm=nm, num=num, den=den, e=ei, sl=sl, tg=tg):
                    rd = rd64[e % 2]
                    P.ts("dve", rd, den, 1.0e-18, ALU.max)
                    P.recip_act(rd, rd)
                    if nm in dbg_br:
                        P.tt("dve", ctmp[e % 2], rd, num, ALU.mult)
                        P.dma(dbg_br[nm][h, :, sl], ctmp[e % 2])
                    gr = _grep(P, bi, h, tg, e)
                    P.tt("dve", rd, rd, gr, ALU.mult)
                    P.tt("dve", ctmp[e % 2], rd, num, ALU.mult)
                    P.tt("pool", OC[:, h, :], OC[:, h, :], ctmp[e % 2], ALU.add)
                    if nm == "win":
                        ob = ocb[h % 2]
                        P.cp("act", ob, OC[:, h, :])
                        P.dma(P.ocT[h, :, sl], ob)

                for ii, kt in enumerate(kts):
                    ks = slice(kt * 128, (kt + 1) * 128)
                    if kt >= 4 * tg:
                        mask = P.cmask[:, kt - 4 * tg, :]
                    elif nm == "win":
                        mask = P.wmask[:, kt - (4 * tg - 4), :]
                    else:
                        mask = None
                    last = ii == len(kts) - 1
                    if nm == "slc":
                        tiles.append(dict(kT=KS[:, g, ks], qT=QA[:, h, sl], vT=VS[:, kt, g * 64:(g + 1) * 64], mask=mask,
                                          num=num, den=den, first=ii == 0, last=last, post=post if last else None))
                    else:
                        tiles.append(dict(kT=KW[0:64, g, ks], qT=QA[0:64, h, sl], vT=VW[:, kt, g * 64:(g + 1) * 64], mask=mask,
                                          num=num, den=den, first=ii == 0, last=last, post=post if last else None))
                ei += 1
        _run_tiles(P, tiles)
    P.top = mark


def stage_moba(P, l):
    I = P.inp
    mark = P.top
    P.load_attn_consts()
    QB = P.sb([128, 8, SEQ], BF16)
    KB = P.sb([128, 8, SEQ], BF16)
    VB = P.sb([128, 16, 512], BF16)
    P.ms("pool", QB[64:128], 0.0)
    P.ms("pool", KB[64:128], 0.0)
    for h in range(8):
        P.dma(KB[64:72, h, :], I["k_mobaug"])
    mA = P.top
    P.rt1 = [P.sb([64, 512], F32) for _ in range(2)]
    P.rt2 = [P.sb([64, 512], F32) for _ in range(2)]
    wbl = [P.sb([128, 8, 128], BF16) for _ in range(4)]
    cnt = 0
    for nm, dstT in (("d_q", QB), ("d_k", KB)):
        for j in range(4):
            w1 = P.load_blk(l, "%s%d" % (nm, j), wbl[(2 * j) % 4])
            w2 = P.load_blk(l, "%s_sw%d" % (nm, j), wbl[(2 * j + 1) % 4])
            for tg in range(4):
                p1 = P.bank(0 + 2 * (cnt % 2))
                p2 = P.bank(1 + 2 * (cnt % 2))
                P.proj(p1, w1, tg)
                P.proj(p2, w2, tg)
                sl = slice(tg * 512, (tg + 1) * 512)
                _rope_evac(P, p1, p2, slice(0, 64), dstT[0:64, 2 * j, sl], tg, cnt)
                _rope_evac(P, p1, p2, slice(64, 128), dstT[0:64, 2 * j + 1, sl], tg, cnt + 1)
                cnt += 1
    wv = P.sb([128, 8, 512], BF16)
    wv4 = wv.rearrange("p k (j c) -> p k j c", j=4)
    for j in range(4):
        P.dma(wv4[:, :, j, :], I["win%d" % l][BLK["d_v%d" % j]], q="pool")
    for t16 in range(16):
        ps = P.bank(4 + (t16 % 2))
        for k in range(8):
            P.mm(ps, P.hT[:, k, t16 * 128:(t16 + 1) * 128], wv[:, k, :], start=(k == 0), stop=(k == 7))
        P.cp("act" if t16 % 2 == 0 else "dve", VB[:, t16, :], ps)
    if "stop_mproj" in P.debug:
        P.top = mark
        return
    P.top = mA
    km = P.sb([64, 8, 8], F32)
    kmb = P.sb([64, 8, 32], BF16)
    P.ms("pool", kmb, 0.0)
    for h in range(8):
        P.red("dve", km[:, h, :], KB[0:64, h, :].rearrange("p (n s) -> p n s", s=256), ALU.add)
    P.ts("dve", kmb[:, :, 0:8], km, float(1.0 / 256), ALU.mult)
    mobadd = P.sb([128, 8, 8], F32)
    P.dma(mobadd, I["k_mobadd"])
    notown = P.sb([128, 8, 8], F32)
    P.dma(notown, I["k_notown"])
    SBq = [P.sb([128, 8, 128], BF16) for _ in range(2)]
    P.ms("pool", SBq[0], 0.0)
    P.ms("pool", SBq[1], 0.0)
    vv = [P.sb([128, 8, 8], F32) for _ in range(2)]
    M8 = [P.sb([128, 8, 8], F32) for _ in range(2)]
    cc = [P.sb([128, 8, 8], F32) for _ in range(2)]
    for t16 in range(8, 16):
        own = t16 // 2
        b = t16 % 2
        ts_ = slice(t16 * 128, (t16 + 1) * 128)
        g_ps = P.bank(6 + b)[:, 0:256].rearrange("p (h n) -> p h n", h=8)
        for h in range(8):
            P.mm(g_ps[:, h, :], QB[0:64, h, ts_], kmb[:, h, :])
        v = vv[b]
        P.tt("dve", v, g_ps[:, :, 0:8], mobadd[:, own, :].unsqueeze(1).to_broadcast([128, 8, 8]), ALU.add)
        for h in range(8):
            P.S.emit("dve", lambda e, o=M8[b][:, h, :], i=v[:, h, :]: e.max(out=o, in_=i), w=[M8[b][:, h, :]], r=[v[:, h, :]])
        P.tt("dve", cc[b], v, M8[b][:, :, 2:3].to_broadcast([128, 8, 8]), ALU.is_ge)
        P.ts("dve", cc[b], cc[b], 1.0, ALU.subtract)
        P.tt("dve", SBq[b][:, :, 64:72], cc[b], notown[:, own, :].unsqueeze(1).to_broadcast([128, 8, 8]), ALU.mult)
        for h in range(8):
            bt = P.bank(0 + (h // 4))[:, (h % 4) * 128:(h % 4 + 1) * 128]
            P.mm(bt, SBq[b][:, h, :], P.ident_b)
        for hb in range(2):
            src = P.bank(hb)[64:72, :].rearrange("p (h t) -> p h t", h=4)
            P.cp("act" if hb == 0 else "dve", QB[64:72, 4 * hb:4 * hb + 4, ts_], src)
    if "stop_mgate" in P.debug:
        P.top = mark
        return
    P.Ebuf = [P.sb([128, 512], BF16) for _ in range(3)]
    rd64 = [P.sb([64, 512], F32) for _ in range(2)]
    odb = [P.sb([64, 512], BF16) for _ in range(2)]
    P.tile_ctr = 0
    ei = 0
    for tg in range(4):
        sl = slice(tg * 512, (tg + 1) * 512)
        tiles = []
        for h in range(8):
            num = P.bank(2 + 2 * (ei % 2))[0:64]
            den = P.bank(3 + 2 * (ei % 2))[0:64]
            kts = list(range(0, 4 * tg + 4))

            def post(h=h, num=num, den=den, e=ei, sl=sl):
                rd = rd64[e % 2]
                P.ts("dve", rd, den, 1.0e-18, ALU.max)
                P.recip_act(rd, rd)
                P.tt("dve", odb[e % 2], rd, num, ALU.mult)
                P.dma(P.odT[h, :, sl], odb[e % 2])

            for ii, kt in enumerate(kts):
                ks = slice(kt * 128, (kt + 1) * 128)
                mask = P.cmask[:, kt - 4 * tg, :] if kt >= 4 * tg else None
                last = ii == len(kts) - 1
                tiles.append(dict(kT=KB[:, h, ks], qT=QB[:, h, sl], vT=VB[:, kt, h * 64:(h + 1) * 64], mask=mask,
                                  num=num, den=den, first=ii == 0, last=last, post=post if last else None))
            ei += 1
        _run_tiles(P, tiles)
    P.top = mark


def stage_merge(P, l, xsrc, xdst):
    I = P.inp
    mark = P.top
    WO = P.sb([128, 8, DM], BF16)
    wst = [P.sb([128, DM], F32) for _ in range(2)]
    for k in range(8):
        P.dma(wst[k % 2], I["wo%d" % l][:, k, :])
        P.tt("dve" if k % 2 == 0 else "pool", WO[:, k, :], wst[k % 2], P.gt1, ALU.mult)
    AM = P.sb([128, 4, 512], BF16)
    BM = P.sb([128, 4, 512], BF16)
    OCt = P.sb([64, 8, 512], BF16)
    ODt = P.sb([64, 8, 512], BF16)
    MG = P.sb([128, 8, 512], BF16)
    gw = [[P.sb([128, 8, 128], BF16) for _ in range(4)] for _ in range(2)]
    wa = [P.sb([128, 4, 128], BF16) for _ in range(2)]
    wb = [P.sb([128, 4, 128], BF16) for _ in range(2)]
    wc = [P.sb([64, 8, 128], BF16) for _ in range(2)]
    wd = [P.sb([64, 8, 128], BF16) for _ in range(2)]
    sg = [P.sb([128, 512], F32) for _ in range(2)]
    acc = [P.sb([128, 512], F32) for _ in range(2)]
    tmp = [P.sb([128, 512], F32) for _ in range(2)]
    xt = [P.sb([128, DM], F32) for _ in range(2)]
    ocv = P.ocT.rearrange("h p t -> p h t")
    odv = P.odT.rearrange("h p t -> p h t")
    it = 0
    for tg in range(4):
        sl = slice(tg * 512, (tg + 1) * 512)
        P.dma(AM, P.amixT[:, :, sl])
        P.dma(BM, P.bmixT[:, :, sl])
        P.dma(OCt, ocv[:, :, sl])
        P.dma(ODt, odv[:, :, sl])
        for m in range(8):
            b = it % 2
            it += 1
            for br in range(4):
                P.load_blk(l, "merge%d" % (br * 8 + m), gw[b][br])
            P.dma(wa[b], I["a_out%d" % l][m], q="pool")
            P.dma(wb[b], I["b_out%d" % l][m], q="pool")
            P.dma(wc[b], I["c_out%d" % l][m], q="pool")
            P.dma(wd[b], I["d_out%d" % l][m], q="pool")
            for br in range(4):
                g_ps = P.bank(2 * (br % 2))
                y_ps = P.bank(2 * (br % 2) + 1)
                P.proj(g_ps, gw[b][br], tg)
                P.act(sg[br % 2], g_ps, AF.Sigmoid)
                if br == 0:
                    for k in range(4):
                        P.mm(y_ps, wa[b][:, k, :], AM[:, k, :], start=(k == 0), stop=(k == 3))
                elif br == 1:
                    for k in range(4):
                        P.mm(y_ps, wb[b][:, k, :], BM[:, k, :], start=(k == 0), stop=(k == 3))
                elif br == 2:
                    for h in range(8):
                        P.mm(y_ps, wc[b][:, h, :], OCt[:, h, :], start=(h == 0), stop=(h == 7))
                else:
                    for h in range(8):
                        P.mm(y_ps, wd[b][:, h, :], ODt[:, h, :], start=(h == 0), stop=(h == 7))
                if br == 0:
                    P.tt("dve", acc[b], sg[br % 2], y_ps, ALU.mult)
                else:
                    P.tt("dve", tmp[br % 2], sg[br % 2], y_ps, ALU.mult)
                    P.tt("pool", acc[b], acc[b], tmp[br % 2], ALU.add)
            P.cp("act", MG[:, m, :], acc[b])
        for t4 in range(4):
            x_ = xt[t4 % 2]
            r0 = tg * 512 + t4 * 128
            P.dma(x_, xsrc[r0:r0 + 128, :])
            for half in range(2):
                o_ps = P.bank(4 + half)
                for k in range(8):
                    P.mm(o_ps, MG[:, k, t4 * 128:(t4 + 1) * 128], WO[:, k, half * 512:(half + 1) * 512], start=(k == 0), stop=(k == 7))
                P.tt("dve", x_[:, half * 512:(half + 1) * 512], x_[:, half * 512:(half + 1) * 512], o_ps, ALU.add)
            P.dma(xdst[r0:r0 + 128, :], x_)
    P.top = mark


def _emit_max(P, out, in_):
    P.S.emit("dve", lambda e: e.max(out=out, in_=in_), w=[out], r=[in_])


def _emit_maxidx(P, out, mx, vals):
    P.S.emit("dve", lambda e: e.max_index(out=out, in_max=mx, in_values=vals), w=[out], r=[mx, vals])


def _emit_mrep(P, out, mx, vals):
    P.S.emit("dve", lambda e: e.match_replace(out=out, in_to_replace=mx, in_values=vals, imm_value=-3.0e38), w=[out], r=[mx, vals])


def _top16_multi(P, items):
    for vals, vout, iout, tmp in items:
        _emit_max(P, vout[:, 0:8], vals)
    for vals, vout, iout, tmp in items:
        _emit_maxidx(P, iout[:, 0:8], vout[:, 0:8], vals)
    for vals, vout, iout, tmp in items:
        _emit_mrep(P, tmp, vout[:, 0:8], vals)
    for vals, vout, iout, tmp in items:
        _emit_max(P, vout[:, 8:16], tmp)
    for vals, vout, iout, tmp in items:
        _emit_maxidx(P, iout[:, 8:16], vout[:, 8:16], tmp)


def stage_peer_prep(P, l):
    I = P.inp
    m0 = P.top
    ub = [P.sb([128, DM], BF16) for _ in range(2)]
    uo = [P.sb([128, 8, 128], BF16) for _ in range(2)]
    vst = [P.sb([128, 4, DM], BF16) for _ in range(2)]
    pu = I["pu%d" % l].rearrange("(c i) d -> c i d", i=128)
    pv = I["pv%d" % l].rearrange("(c i) d -> i c d", i=128)
    vbv = P.vbS.rearrange("c i d -> i c d")
    for c in range(128):
        b = c % 2
        P.dma(ub[b], pu[c], q="pool")
        pT = P.bank(6 + b, 1024, BF16)
        for k in range(8):
            P.tr(pT[:, k * 128:(k + 1) * 128], ub[b][:, k * 128:(k + 1) * 128], P.ident_b)
        P.cp("act" if b == 0 else "dve", uo[b], pT.rearrange("p (k i) -> p k i", k=8))
        P.dma(P.utS[c], uo[b])
        if c % 4 == 0:
            vb_ = vst[(c // 4) % 2]
            P.dma(vb_, pv[:, c:c + 4, :], q="pool")
            P.dma(vbv[:, c:c + 4, :], vb_)
    P.top = m0


def stage_peer(P, l, xsrc, xdst, final):
    I = P.inp
    mark = P.top
    skT = P.sb([128, 16, 128], F32)
    P.dma(skT, I["skT%d" % l])
    W_sb = P.sb([128, 256, 128], BF16)
    h2T = P.sb([128, 8, 256], BF16)
    xt2 = [P.sb([128, DM], F32) for _ in range(2)]
    uTb = [P.sb([128, 2, 8, 128], BF16) for _ in range(2)]
    vbb = [P.sb([128, 2, DM], BF16) for _ in range(2)]
    utv = P.utS.rearrange("c p k i -> p c k i")
    vbv2 = P.vbS.rearrange("c i d -> i c d")
    I3T = P.sb([128, 3, 256], F32)
    Gb = [P.sb([128, 256], BF16) for _ in range(2)]
    Wa = [P.sb([128, 256], BF16) for _ in range(3)]
    etmp = [P.sb([128, 512], F32) for _ in range(2)]
    if final:
        fng = P.sb([128, DM], F32)
        P.dma(fng, I["fng"].partition_broadcast(128), q="pool")
        ot = [P.sb([128, DM], F32) for _ in range(2)]
    for gi in range(8):
        if "peer_prep_only" in P.debug or ("peer_1group" in P.debug and gi >= 1):
            break
        t0 = gi * 256
        for t2 in range(2):
            P.dma(xt2[t2], xsrc[t0 + t2 * 128: t0 + (t2 + 1) * 128, :])
            P.norm_tile(xt2[t2], P.A2, P.modT[:, 24:32], h2T[:, :, t2 * 128:(t2 + 1) * 128], 6 + t2)
        mB = P.top
        sc_sb = P.sb([128, 2, 16, 128], F32)
        mB2 = P.top
        wqb = [P.sb([128, 8, 128], BF16) for _ in range(2)]
        qf = [P.sb([128, 256], F32) for _ in range(2)]
        sq = [P.sb([128, 256], F32) for _ in range(2)]
        rs = [P.sb([128, 256], F32) for _ in range(2)]
        qn = [P.sb([128, 256], F32) for _ in range(2)]
        def q_s1(j):
            b = j % 2
            P.dma(wqb[b], I["wq%d" % l][:, :, j * 128:(j + 1) * 128], q="pool")
            q_ps = P.bank(2 + b)[:, 0:256]
            for k in range(8):
                P.mm(q_ps, wqb[b][:, k, :], h2T[:, k, :], start=(k == 0), stop=(k == 7))
            P.cp("act", qf[b], q_ps)
            P.act(sq[b], q_ps, AF.Square)

        def q_s2(j):
            b = j % 2
            s_ps = P.bank(4 + b)[:, 0:256]
            P.mm(s_ps, P.ones_f, sq[b])
            P.ts("dve", rs[b], s_ps, float(1.0 / 128), ALU.mult, 1.0e-6, ALU.add)
            P.act(rs[b], rs[b], AF.Sqrt)
            P.recip(rs[b], rs[b])
            P.tt("dve", qn[b], qf[b], rs[b], ALU.mult)
            c_ps = P.bank(0 + b)[:, 0:256]
            for t2 in range(2):
                P.mm(c_ps[:, t2 * 128:(t2 + 1) * 128], qn[b][:, t2 * 128:(t2 + 1) * 128], skT[:, j, :])
            P.cp("dve", sc_sb[:, :, j, :], c_ps.rearrange("p (a n) -> p a n", a=2))

        q_s1(0)
        for j in range(16):
            if j + 1 < 16:
                q_s1(j + 1)
            q_s2(j)
        if ("peer_sc%d" % l) in P.debug and gi == 0:
            dd = P.dram("dbg_peer_sc%d" % l, [128, 2, 16, 128], F32)
            P.dma(dd, sc_sb)
        P.top = mB2
        V = P.sb([128, 16, 16], F32)
        IDX = P.sb([128, 16, 16], U32)
        IDXf = P.sb([128, 16, 16], F32)
        tmpa = [P.sb([128, 128], F32) for _ in range(4)]
        cand = P.sb([128, 8, 256], F32)
        tmpc = [P.sb([128, 256], F32) for _ in range(4)]
        TOP = P.sb([128, 8, 16], F32)
        CI = P.sb([128, 8, 16], U32)
        AKu = P.sb([128, 8, 16], U32)
        BKu = P.sb([128, 8, 16], U32)
        AK = P.sb([128, 8, 16], F32)
        BK = P.sb([128, 8, 16], F32)
        oh = P.sb([128, 128, 16], F32)
        I1S = P.sb([128, 128], F32)
        I2S = P.sb([128, 128], F32)
        dsm = P.sb([128, 8, 16], F32)
        ssum = P.sb([128, 8], F32)
        Gm = P.sb([128, 8, 16], F32)
        iota16 = P.iota[:, 0:16].unsqueeze(1).unsqueeze(1).to_broadcast([128, 8, 16, 16])
        for t2 in range(2):
            if "peer_notopk" in P.debug:
                break
            for j0 in range(0, 16, 4):
                _top16_multi(P, [(sc_sb[:, t2, j, :], V[:, j, :], IDX[:, j, :], tmpa[j % 4]) for j in range(j0, j0 + 4)])
            P.cp("dve", IDXf, IDX)
            V4 = V.rearrange("p (h two) a -> p h two a", two=2)
            P.tt("dve", cand.rearrange("p h (a b) -> p h a b", a=16),
                 V4[:, :, 0, :].unsqueeze(3).to_broadcast([128, 8, 16, 16]),
                 V4[:, :, 1, :].unsqueeze(2).to_broadcast([128, 8, 16, 16]), ALU.add)
            for h0 in range(0, 8, 4):
                _top16_multi(P, [(cand[:, h, :], TOP[:, h, :], CI[:, h, :], tmpc[h % 4]) for h in range(h0, h0 + 4)])
            P.ts("dve", AKu, CI, 4, ALU.logical_shift_right)
            P.ts("dve", BKu, CI, 15, ALU.bitwise_and)
            P.cp("dve", AK, AKu)
            P.cp("dve", BK, BKu)
            IDX4 = IDXf.rearrange("p (h two) a -> p h two a", two=2)
            oh4 = oh.rearrange("p (h k) a -> p h k a", h=8)
            for XK, pidx, OUT in ((AK, 0, I1S), (BK, 1, I2S)):
                P.tt("dve", oh4, iota16, XK.unsqueeze(3).to_broadcast([128, 8, 16, 16]), ALU.is_equal)
                P.tt("dve", oh4, oh4, IDX4[:, :, pidx, :].unsqueeze(2).to_broadcast([128, 8, 16, 16]), ALU.mult)
                P.red("dve", OUT, oh, ALU.add)
            P.tt("dve", dsm, TOP, TOP[:, :, 0:1].to_broadcast([128, 8, 16]), ALU.subtract)
            P.act(dsm, dsm, AF.Exp)
            P.red("dve", ssum, dsm, ALU.add)
            P.recip(ssum, ssum)
            P.tt("dve", Gm, dsm, ssum.unsqueeze(2).to_broadcast([128, 8, 16]), ALU.mult)
            tr_ps = P.bank(6)[:, 0:384]
            P.tr(tr_ps[:, 0:128], I1S, P.ident_f)
            P.tr(tr_ps[:, 128:256], I2S, P.ident_f)
            P.tr(tr_ps[:, 256:384], Gm.rearrange("p h k -> p (h k)"), P.ident_f)
            P.cp("act", I3T[:, :, t2 * 128:(t2 + 1) * 128], tr_ps.rearrange("p (a t) -> p a t", a=3))
        if ("peer_sel%d" % l) in P.debug and gi == 0:
            dd = P.dram("dbg_peer_sel%d" % l, [128, 3, 256], F32)
            P.dma(dd, I3T)
        P.top = mB
        mC = P.top
        A = [P.sb([128, 16, 128], BF16) for _ in range(2)]
        B = [P.sb([128, 16, 128], BF16) for _ in range(2)]
        def ab_build(sbi):
            ab = sbi % 2
            tsl = slice(sbi * 16, sbi * 16 + 16)
            P.tt("dve", B[ab], P.iota.unsqueeze(1).to_broadcast([128, 16, 128]),
                 I3T[:, 1, tsl].unsqueeze(2).to_broadcast([128, 16, 128]), ALU.is_equal)
            for tl in range(16):
                t = sbi * 16 + tl
                P.ts("dve", A[ab][:, tl, :], P.iota, I3T[:, 0, t:t + 1], ALU.is_equal, I3T[:, 2, t:t + 1], ALU.mult)

        NSB = 0 if "peer_nowb" in P.debug else 16
        if NSB:
            ab_build(0)
        for sbi in range(NSB):
            ab = sbi % 2
            for q4 in range(4):
                wb_ = q4
                w_ps = P.bank(wb_)
                for u in range(4):
                    tl = q4 * 4 + u
                    P.mm(w_ps[:, u * 128:(u + 1) * 128], B[ab][:, tl, :], A[ab][:, tl, :])
            if sbi + 1 < NSB:
                ab_build(sbi + 1)
            for q4 in range(4):
                wb_ = q4
                w_ps = P.bank(wb_)
                tb = sbi * 16 + q4 * 4
                P.cp("act" if wb_ % 2 == 0 else "dve", W_sb[:, tb:tb + 4, :],
                     w_ps.rearrange("p (t c) -> p t c", t=4))
        P.top = mC
        SB3 = (4, 5, 6)

        def s_mm(c):
            if c % 2 == 0:
                P.dma(uTb[(c // 2) % 2], utv[:, c:c + 2])
                P.dma(vbb[(c // 2) % 2], vbv2[:, c:c + 2, :])
            ub_ = uTb[(c // 2) % 2][:, c % 2]
            S_ps = P.bank(SB3[c % 3])[:, 0:256]
            for k in range(8):
                P.mm(S_ps, ub_[:, k, :], h2T[:, k, :], start=(k == 0), stop=(k == 7))

        NC_ = 0 if "peer_nomain" in P.debug else 128
        if NC_:
            s_mm(0)
        for c in range(NC_):
            if c + 1 < NC_:
                s_mm(c + 1)
            vb_ = vbb[(c // 2) % 2][:, c % 2]
            S_ps = P.bank(SB3[c % 3])[:, 0:256]
            P.act(Gb[c % 2], S_ps, AF.Gelu)
            wa = Wa[c % 3]
            P.tt("dve" if c % 2 == 0 else "pool", wa, Gb[c % 2], W_sb[:, :, c], ALU.mult)
            for t2 in range(2):
                for half in range(2):
                    P.mm(P.bank(t2 * 2 + half), wa[:, t2 * 128:(t2 + 1) * 128], vb_[:, half * 512:(half + 1) * 512],
                         start=(c == 0), stop=(c == 127))
        for t2 in range(2):
            x_ = xt2[t2]
            for half in range(2):
                hs = slice(half * 512, (half + 1) * 512)
                e_ = etmp[half]
                P.tt("dve", e_, P.gt2[:, hs], P.bank(t2 * 2 + half), ALU.mult)
                P.tt("pool", x_[:, hs], x_[:, hs], e_, ALU.add)
            r0 = t0 + t2 * 128
            if not final:
                P.dma(xdst[r0:r0 + 128, :], x_)
            else:
                mF = P.top
                junk = P.sb([128, DM], BF16)
                ssq = P.sb([128, 1], F32)
                rstd = P.sb([128, 1], F32)
                P.act(junk, x_, AF.Square, accum=ssq)
                P.act(rstd, ssq, AF.Sqrt, scale=float(1.0 / DM), bias=P.eps_ap[:, 0:1])
                P.recip(rstd, rstd)
                o_ = ot[t2]
                P.ts("dve", o_, x_, rstd[:, 0:1], ALU.mult)
                P.tt("pool", o_, o_, fng, ALU.mult)
                P.dma(xdst[r0:r0 + 128, :], o_)
                P.top = mF
    P.top = mark


def kernel(**inputs):
    inp = {k: np.asarray(v) for k, v in inputs.items()}
    shared = _prep_shared(inp)
    cores = [core_inputs(inp, b) for b in range(NCORES)]
    P = build(input_shapes(shared, cores[0]), upto="all")
    in_maps = []
    for b in range(NCORES):
        m = dict(shared)
        m.update(cores[b])
        in_maps.append(m)
    res = run_bass_kernel_spmd(P.nc, in_maps, core_ids=list(range(NCORES)))
    out = np.stack([np.asarray(res.results[b]["out"], dtype=np.float32) for b in range(NCORES)], axis=0)
    return out
```

```python
import os
import numpy as np
import ml_dtypes
import concourse.bass as bass
import concourse.mybir as mybir
from concourse.bass_utils import run_bass_kernel_spmd

F32 = mybir.dt.float32
BF16 = mybir.dt.bfloat16
I32 = mybir.dt.int32
U32 = mybir.dt.uint32
U8 = mybir.dt.uint8
ALU = mybir.AluOpType
AF = mybir.ActivationFunctionType
AX = mybir.AxisListType
_DTSZ = {F32: 4, BF16: 2, I32: 4, U32: 4, U8: 1}
NPBF = ml_dtypes.bfloat16

SEQ = 2048
DM = 1024
DEPTH = 2
NCORES = 8
BIG = 1.0e30
NEGB = 30000.0


def _box(ap):
    t = ap.tensor
    sz = _DTSZ[ap.dtype]
    pairs = list(ap.ap)
    off = int(ap.offset)
    if type(t).__name__ == "DRamTensorHandle":
        span = 1
        for st, cnt in pairs:
            span += (cnt - 1) * abs(st)
        return (t.name, 0, 1, off * sz, (off + span) * sz)
    pstride = 1
    for s in list(t.shape)[1:]:
        pstride *= s
    p0 = off // pstride
    f0 = off % pstride
    if pairs and pairs[0][0] == pstride:
        pc = pairs[0][1]
        rest = pairs[1:]
    else:
        pc = 1
        rest = pairs
    span = 1
    for st, cnt in rest:
        span += (cnt - 1) * abs(st)
    return (t.name, p0, p0 + pc, f0 * sz, (f0 + span) * sz)


class Sched:
    ENG = ("pe", "act", "dve", "pool", "sp")

    def __init__(self, nc, n_dma_slots=8):
        self.nc = nc
        self.ops = []
        self.rec = {}
        self.n_dma_slots = n_dma_slots

    def emit(self, eng, fn, w=(), r=(), dma=False):
        oid = len(self.ops)
        deps = set()
        for ap in r:
            self._access(oid, eng, dma, ap, False, deps)
        for ap in w:
            self._access(oid, eng, dma, ap, True, deps)
        deps.discard(oid)
        self.ops.append(dict(eng=eng, fn=fn, deps=deps, dma=dma))
        return oid

    def _access(self, oid, eng, dma, ap, is_write, deps):
        name, p0, p1, b0, b1 = _box(ap)
        lst = self.rec.get(name)
        if lst is None:
            lst = self.rec[name] = []
        keep = []
        for rc in lst:
            q0, q1, c0, c1, rid, rw, reng, rdma = rc
            if q1 <= p0 or p1 <= q0 or c1 <= b0 or b1 <= c0 or rid == oid:
                keep.append(rc)
                continue
            if is_write or rw:
                deps.add(rid)
            covered = (p0 <= q0 and q1 <= p1 and b0 <= c0 and c1 <= b1)
            if is_write and covered:
                continue
            if (not is_write) and (not rw) and covered and reng == eng and not dma and not rdma:
                continue
            keep.append(rc)
        keep.append((p0, p1, b0, b1, oid, is_write, eng, dma))
        self.rec[name] = keep

    def finalize(self):
        nc = self.nc
        ops = self.ops
        needed = set()
        for o in ops:
            nd = set()
            for d in o["deps"]:
                po = ops[d]
                if po["eng"] == "pe" and o["eng"] == "pe" and not po["dma"] and not o["dma"]:
                    continue
                nd.add(d)
            o["deps"] = nd
            needed |= nd
        engobj = dict(pe=nc.tensor, act=nc.scalar, dve=nc.vector, pool=nc.gpsimd, sp=nc.sync)
        sems = {e: nc.alloc_semaphore("s_" + e) for e in self.ENG}
        dma_sems = {e: [nc.alloc_semaphore("d_%s%d" % (e, i)) for i in range(self.n_dma_slots)] for e in ("sp", "pool")}
        cnt = {e: 0 for e in self.ENG}
        slot_next = {e: 0 for e in dma_sems}
        slot_val = {e: [0] * self.n_dma_slots for e in dma_sems}
        waited = {e: {} for e in self.ENG}
        sig = {}
        n_wait = 0
        for i, o in enumerate(ops):
            e = o["eng"]
            eo = engobj[e]
            req = {}
            for d in o["deps"]:
                k, so, v = sig[d]
                if k not in req or req[k][1] < v:
                    req[k] = (so, v)
            if o["dma"]:
                s = slot_next[e]
                slot_next[e] = (s + 1) % self.n_dma_slots
                k = ("d", e, s)
                pv = slot_val[e][s]
                if pv > 0 and (k not in req or req[k][1] < pv):
                    req[k] = (dma_sems[e][s], pv)
            for k2, (so, v) in req.items():
                if waited[e].get(k2, 0) >= v:
                    continue
                eo.wait_ge(so, v)
                waited[e][k2] = v
                n_wait += 1
            inst = o["fn"](eo)
            if o["dma"]:
                slot_val[e][s] += 16
                inst.then_inc(dma_sems[e][s], 16)
                sig[i] = (k, dma_sems[e][s], slot_val[e][s])
            elif i in needed:
                cnt[e] += 1
                inst.then_inc(sems[e], 1)
                sig[i] = (("e", e), sems[e], cnt[e])
            o["fn"] = None
        eo = engobj["sp"]
        for e in dma_sems:
            for s in range(self.n_dma_slots):
                if slot_val[e][s] > 0:
                    eo.wait_ge(dma_sems[e][s], slot_val[e][s])
        for e in self.ENG:
            if cnt[e] > 0:
                eo.wait_ge(sems[e], cnt[e])
        return dict(n_ops=len(ops), n_wait=n_wait, cnt=dict(cnt))


SEC = dict(a_b=0, a_c=512, a_x=1024, b_a=1536, b_g=2048, c_q=2560, c_kc=3072, c_vc=3200, c_ks=3328,
           c_vs=3456, c_kw=3584, c_vw=3712, c_gate=3840, d_q=3864, d_k=4376, d_v=4888, merge=5400)


def _blocks():
    blk = {}
    cols = []

    def add(name, c0, n=128, swap=False):
        idx = np.arange(c0, c0 + 128)
        if n < 128:
            idx = np.where(np.arange(128) < n, idx, -1)
        if swap:
            idx = idx.reshape(2, 2, 32)[:, ::-1, :].reshape(128)
        blk[name] = len(cols)
        cols.append(idx)

    for s in ("a_b", "a_c", "a_x", "b_a", "b_g"):
        for j in range(4):
            add("%s%d" % (s, j), SEC[s] + 128 * j)
    for j in range(4):
        add("c_q%d" % j, SEC["c_q"] + 128 * j)
        add("c_q_sw%d" % j, SEC["c_q"] + 128 * j, swap=True)
    for s in ("c_kc", "c_ks", "c_kw"):
        add(s, SEC[s])
        add(s + "_sw", SEC[s], swap=True)
    for s in ("c_vc", "c_vs", "c_vw"):
        add(s, SEC[s])
    add("c_gate", SEC["c_gate"], n=24)
    for s in ("d_q", "d_k"):
        for j in range(4):
            add("%s%d" % (s, j), SEC[s] + 128 * j)
            add("%s_sw%d" % (s, j), SEC[s] + 128 * j, swap=True)
    for j in range(4):
        add("d_v%d" % j, SEC["d_v"] + 128 * j)
    for j in range(32):
        add("merge%d" % j, SEC["merge"] + 128 * j)
    return blk, np.stack(cols)


BLK, BLKCOLS = _blocks()
NBLK = BLKCOLS.shape[0]


def _consts():
    c = {}
    c["ident"] = np.eye(128, dtype=np.float32)
    kl = np.arange(128)[:, None]
    ql = np.arange(512)[None, :]
    cm = np.stack([(kl + 128 * o <= ql) for o in range(4)], axis=1).astype(np.float32)
    c["cmask"] = cm.astype(NPBF)
    c["wmask"] = (1.0 - cm).astype(NPBF)
    n = np.arange(128)[:, None]
    t = np.arange(SEQ)[None, :]
    c["cmpmask"] = ((16 * n + 31 <= t) & (n < 127)).astype(NPBF)
    j = np.arange(32)[None, :]
    cs = 16 * n
    ov = ((cs < 64 * j + 64) & (cs + 32 > 64 * j) & (n < 127)).astype(np.float32)
    c["overlap"] = ov
    tt = np.arange(SEQ)
    cur = tt // 64
    jj = np.arange(32)[None, :]
    forced = (jj == 0) | (jj == cur[:, None]) | (jj == cur[:, None] - 1)
    fut = jj > cur[:, None]
    add = np.where(forced, BIG, np.where(fut, -BIG, 0.0)).astype(np.float32)
    c["slcadd"] = np.ascontiguousarray(add.reshape(16, 128, 32).transpose(1, 0, 2))
    keys = np.arange(SEQ)[None, :]
    c["slcaug"] = (NEGB * (keys // 64 == np.arange(32)[:, None])).astype(NPBF)
    c["mobaug"] = (NEGB * (keys // 256 == np.arange(8)[:, None])).astype(NPBF)
    own = np.arange(8)[:, None]
    nn = np.arange(8)[None, :]
    ma = np.where(nn < own, 0.0, -BIG).astype(np.float32)
    c["mobadd"] = np.ascontiguousarray(np.broadcast_to(ma[None], (128, 8, 8))).astype(np.float32)
    c["notown"] = np.ascontiguousarray(np.broadcast_to((nn != own).astype(np.float32)[None], (128, 8, 8)))
    sel = np.zeros((32, 24, 64), np.float32)
    for i in range(24):
        sel[i, i, :] = 1.0
    c["selall"] = sel.astype(NPBF)
    c["iota"] = np.ascontiguousarray(np.broadcast_to(np.arange(128, dtype=np.float32)[None], (128, 128)))
    inv = 1.0 / (10000.0 ** (np.arange(0, 64, 2, dtype=np.float32) / np.float32(64)))
    inv = inv.astype(np.float32)
    c["invf"] = np.concatenate([inv, inv])[:, None].astype(np.float32)
    c["sgn"] = np.concatenate([-np.ones(32), np.ones(32)])[:, None].astype(np.float32)
    return c


CONSTS = _consts()


def _prep_shared(inp):
    d = {}
    L = DEPTH

    def fm(v, nk):
        return np.ascontiguousarray(v.reshape(nk, 128).T)

    w_in = inp["w_in"]
    wpad = np.concatenate([w_in, np.zeros((L, DM, 1), np.float32)], axis=2)
    cols = np.where(BLKCOLS < 0, w_in.shape[2], BLKCOLS)
    for l in range(L):
        g = wpad[l][:, cols]
        d["win%d" % l] = np.ascontiguousarray(g.reshape(8, 128, NBLK, 128).transpose(2, 1, 0, 3))
        mw = inp["mod_w"][l].reshape(8, 128, 12, 512).transpose(2, 1, 0, 3)
        d["modw%d" % l] = np.ascontiguousarray(mw)
        d["modbT%d" % l] = fm(inp["mod_b"][l], 48)
        d["modbrow%d" % l] = np.ascontiguousarray(inp["mod_b"][l][None, :])
        d["gmix%d" % l] = fm(inp["norm_mix_g"][l], 8)
        d["gffn%d" % l] = fm(inp["norm_ffn_g"][l], 8)
        d["acw%d" % l] = np.ascontiguousarray(inp["a_conv_w"][l].reshape(3, 4, 128).transpose(2, 1, 0))
        d["bcw%d" % l] = np.ascontiguousarray(inp["b_conv_w"][l].reshape(31, 4, 128).transpose(2, 1, 0))
        d["bcb%d" % l] = fm(inp["b_conv_b"][l], 4)
        d["blg%d" % l] = fm(inp["b_ln_g"][l], 4)
        d["blb%d" % l] = fm(inp["b_ln_b"][l], 4)
        for nm in ("a_out", "b_out"):
            w = inp[nm][l].reshape(4, 128, 8, 128).transpose(2, 1, 0, 3)
            d["%s%d" % (nm, l)] = np.ascontiguousarray(w)
        for nm in ("c_out", "d_out"):
            w = inp[nm][l].reshape(8, 64, 8, 128).transpose(2, 1, 0, 3)
            d["%s%d" % (nm, l)] = np.ascontiguousarray(w)
        d["wo%d" % l] = np.ascontiguousarray(inp["w_o"][l].reshape(8, 128, 1024).transpose(1, 0, 2))
        d["wq%d" % l] = np.ascontiguousarray(inp["peer_wq"][l].reshape(8, 128, 2048).transpose(1, 0, 2))
        sk = inp["peer_subkeys"][l].reshape(16, 128, 128)
        d["skT%d" % l] = np.ascontiguousarray(sk.transpose(2, 0, 1))
        d["pu%d" % l] = inp["peer_u"][l]
        d["pv%d" % l] = inp["peer_v"][l]
        w1 = inp["c_cmp_w1"][l].reshape(2, 32, 64, 128).transpose(2, 0, 1, 3)
        d["w1r%d" % l] = np.ascontiguousarray(w1)
        d["w2r%d" % l] = np.ascontiguousarray(inp["c_cmp_w2"][l].transpose(1, 0, 2))
        d["posT%d" % l] = np.ascontiguousarray(inp["c_cmp_pos"][l].transpose(2, 0, 1))
    d["fng"] = np.ascontiguousarray(inp["final_norm_g"][None, :])
    for k, v in CONSTS.items():
        d["k_" + k] = v
    return d


class Prog:
    def __init__(self, shapes, debug=()):
        self.nc = nc = bass.Bass("TRN2", target_bir_lowering=False)
        self.S = Sched(nc)
        self.debug = set(debug)
        self.inp = {}
        for name, (shape, dt) in shapes.items():
            self.inp[name] = nc.dram_tensor(name, list(shape), dt, kind="ExternalInput").ap()
        self.outs = {}
        self.arena = nc.alloc_sbuf_tensor("arena", [128, 207 * 1024], U8)
        self.top = 0
        self.psum = nc.alloc_psum_tensor("ps", [128, 4096], F32)

    def dram(self, name, shape, dt):
        t = self.nc.dram_tensor(name, list(shape), dt, kind="ExternalOutput").ap()
        self.outs[name] = t
        return t

    def sb(self, shape, dt):
        n = int(np.prod(shape[1:])) * _DTSZ[dt]
        a = self.arena[:, self.top:self.top + n].bitcast(dt)
        self.top += (n + 63) // 64 * 64
        self.maxtop = max(getattr(self, 'maxtop', 0), self.top)
        assert self.top <= 207 * 1024, "SBUF arena overflow %d" % self.top
        if len(shape) == 3:
            a = a.rearrange("p (a b) -> p a b", a=shape[1])
        elif len(shape) == 4:
            a = a.rearrange("p (a b c) -> p a b c", a=shape[1], b=shape[2])
        return a[: shape[0]] if shape[0] < 128 else a

    def bank(self, b, n=512, dt=F32, parts=128, off=0):
        a = self.psum[:, b * 512 + off: b * 512 + off + (n if dt == F32 else n // 2)]
        if dt != F32:
            a = a.bitcast(dt)
        return a[:parts] if parts < 128 else a

    def dma(self, out, in_, q="sp"):
        self.S.emit(q, lambda e: e.dma_start(out=out, in_=in_), w=[out], r=[in_], dma=True)

    def mm(self, out, lhsT, rhs, start=True, stop=True, xw=()):
        self.S.emit("pe", lambda e: e.matmul(out, lhsT, rhs, start=start, stop=stop), w=[out] + list(xw), r=[lhsT, rhs])

    def tr(self, out, in_, ident, xw=()):
        self.S.emit("pe", lambda e: e.transpose(out, in_, ident), w=[out] + list(xw), r=[in_, ident])

    def act(self, out, in_, func, bias=None, scale=None, accum=None, xr=()):
        kw = {}
        rr = [in_] + list(xr)
        ww = [out]
        if bias is not None:
            kw["bias"] = bias
            if not isinstance(bias, (int, float)):
                rr.append(bias)
        if scale is not None:
            kw["scale"] = scale
            if not isinstance(scale, (int, float)):
                rr.append(scale)
        if accum is not None:
            kw["accum_out"] = accum
            ww.append(accum)
        self.S.emit("act", lambda e: e.activation(out=out, in_=in_, func=func, **kw), w=ww, r=rr)

    def tt(self, eng, out, in0, in1, op):
        self.S.emit(eng, lambda e: e.tensor_tensor(out=out, in0=in0, in1=in1, op=op), w=[out], r=[in0, in1])

    def ts(self, eng, out, in0, s1, op0, s2=None, op1=None, xr=()):
        rr = [in0] + list(xr)
        for s in (s1, s2):
            if s is not None and not isinstance(s, (int, float)):
                rr.append(s)
        if op1 is None:
            self.S.emit(eng, lambda e: e.tensor_scalar(out=out, in0=in0, scalar1=s1, scalar2=None, op0=op0), w=[out], r=rr)
        else:
            self.S.emit(eng, lambda e: e.tensor_scalar(out=out, in0=in0, scalar1=s1, scalar2=s2, op0=op0, op1=op1), w=[out], r=rr)

    def stt(self, eng, out, in0, scalar, in1, op0, op1):
        rr = [in0, in1]
        if not isinstance(scalar, (int, float)):
            rr.append(scalar)
        self.S.emit(eng, lambda e: e.scalar_tensor_tensor(out=out, in0=in0, scalar=scalar, in1=in1, op0=op0, op1=op1), w=[out], r=rr)

    def cp(self, eng, out, in_, xr=()):
        if eng == "act":
            self.S.emit("act", lambda e: e.copy(out=out, in_=in_), w=[out], r=[in_] + list(xr))
        else:
            self.S.emit(eng, lambda e: e.tensor_copy(out=out, in_=in_), w=[out], r=[in_] + list(xr))

    def ms(self, eng, ap, val):
        self.S.emit(eng, lambda e: e.memset(ap, val), w=[ap])

    def red(self, eng, out, in_, op):
        self.S.emit(eng, lambda e: e.tensor_reduce(out=out, in_=in_, axis=AX.X, op=op), w=[out], r=[in_])

    def recip(self, out, in_):
        self.S.emit("dve", lambda e: e.reciprocal(out=out, in_=in_), w=[out], r=[in_])

    def recip_act(self, out, in_):
        self.act(out, in_, AF.Ln)
        self.act(out, out, AF.Exp, scale=-1.0)

    def dbg(self, name, ap, dt=F32):
        if name not in self.debug:
            return
        d = self.dram("dbg_" + name, list(ap.shape), dt)
        self.dma(d, ap)

    def load_consts(self):
        I = self.inp
        self.ident_f = self.sb([128, 128], F32)
        self.dma(self.ident_f, I["k_ident"])
        self.ident_b = self.sb([128, 128], BF16)
        self.cp("dve", self.ident_b, self.ident_f)
        self.ones_f = self.sb([128, 128], F32)
        self.ms("pool", self.ones_f, 1.0)
        self.ones_b = self.sb([128, 64], BF16)
        self.ms("pool", self.ones_b, 1.0)
        self.iota = self.sb([128, 128], F32)
        self.dma(self.iota, I["k_iota"])
        self.eps_ap = self.sb([128, 1], F32)
        self.ms("pool", self.eps_ap, 1.0e-6)
        ct = self.sb([128, 8], F32)
        self.dma(ct, I["cT"])
        self.cond = self.sb([128, 8], F32)
        self.act(self.cond, ct, AF.Silu)
        invf = self.sb([64, 1], F32)
        sgn = self.sb([64, 1], F32)
        self.dma(invf, I["k_invf"])
        self.dma(sgn, I["k_sgn"])
        mark = self.top
        self.cos64 = self.sb([64, SEQ], F32)
        self.sinS = self.sb([64, SEQ], F32)
        self.ropeS = self.dram("s_rope", [2, 64, SEQ], F32)
        posi = self.sb([64, SEQ], I32)
        self.dma(posi, I["pos"].partition_broadcast(64), q="pool")
        ang = self.sb([64, SEQ], F32)
        kf = self.sb([64, SEQ], F32)
        ki = self.sb([64, SEQ], I32)
        r = self.sb([64, SEQ], F32)
        self.cp("dve", ang, posi)
        self.ts("dve", ang, ang, invf[:, 0:1], ALU.mult)
        self.ts("dve", kf, ang, float(1.0 / (2 * np.pi)), ALU.mult)
        self.cp("dve", ki, kf)
        self.cp("dve", kf, ki)
        C1 = 6.28125
        C2 = float(2 * np.pi - 6.28125)
        self.stt("dve", r, kf, -C1, ang, ALU.mult, ALU.add)
        self.stt("dve", r, kf, -C2, r, ALU.mult, ALU.add)
        LIM = 3.1415925
        self.ts("dve", r, r, -LIM, ALU.max, LIM, ALU.min)
        self.act(self.sinS, r, AF.Sin)
        self.ts("dve", self.sinS, self.sinS, sgn[:, 0:1], ALU.mult)
        m = kf
        self.ts("dve", m, r, float(np.pi / 2), ALU.is_gt)
        self.stt("dve", r, m, float(-2 * np.pi), r, ALU.mult, ALU.add)
        self.ts("dve", r, r, float(np.pi / 2), ALU.add, -LIM, ALU.max)
        self.ts("dve", r, r, LIM, ALU.min)
        self.act(self.cos64, r, AF.Sin)
        self.dbg("cos64", self.cos64)
        self.dbg("sinS", self.sinS)
        self.dma(self.ropeS[0], self.cos64)
        self.dma(self.ropeS[1], self.sinS)
        self.top = mark

    def load_attn_consts(self):
        I = self.inp
        self.cmask = self.sb([128, 4, 512], BF16)
        self.dma(self.cmask, I["k_cmask"])
        self.wmask = self.sb([128, 4, 512], BF16)
        self.dma(self.wmask, I["k_wmask"])
        self.cos64 = self.sb([64, SEQ], F32)
        self.sinS = self.sb([64, SEQ], F32)
        self.dma(self.cos64, self.ropeS[0])
        self.dma(self.sinS, self.ropeS[1])

    def stage_mod(self, l):
        I = self.inp
        self.modT = self.sb([128, 48], F32)
        self.gt1 = self.sb([128, DM], F32)
        self.gt2 = self.sb([128, DM], F32)
        self.A1 = self.sb([128, 8], F32)
        self.A2 = self.sb([128, 8], F32)
        mark = self.top
        wst = [self.sb([128, 8, 512], F32) for _ in range(2)]
        modps = self.bank(0, 48)
        gps = {4: self.bank(1), 5: self.bank(2), 10: self.bank(3), 11: self.bank(4)}
        for g in range(12):
            w = wst[g % 2]
            self.dma(w, I["modw%d" % l][g], q="sp" if g % 2 == 0 else "pool")
            for j4 in range(4):
                j = g * 4 + j4
                for k in range(8):
                    self.mm(modps[:, j:j + 1], w[:, k, j4 * 128:(j4 + 1) * 128], self.cond[:, k:k + 1], start=(k == 0), stop=(k == 7))
            if g in gps:
                for k in range(8):
                    self.mm(gps[g], self.cond[:, k:k + 1].to_broadcast([128, 128]), w[:, k, :], start=(k == 0), stop=(k == 7))
        mb = self.sb([128, 48], F32)
        self.dma(mb, I["modbT%d" % l])
        self.tt("dve", self.modT, modps, mb, ALU.add)
        brow = self.sb([128, DM], F32)
        self.dma(brow, I["modbrow%d" % l][:, 2048:3072].partition_broadcast(128), q="pool")
        self.tt("dve", self.gt1[:, 0:512], gps[4], brow[:, 0:512], ALU.add)
        self.tt("dve", self.gt1[:, 512:1024], gps[5], brow[:, 512:1024], ALU.add)
        brow2 = self.sb([128, DM], F32)
        self.dma(brow2, I["modbrow%d" % l][:, 5120:6144].partition_broadcast(128), q="pool")
        self.tt("dve", self.gt2[:, 0:512], gps[10], brow2[:, 0:512], ALU.add)
        self.tt("dve", self.gt2[:, 512:1024], gps[11], brow2[:, 512:1024], ALU.add)
        g1 = self.sb([128, 8], F32)
        g2 = self.sb([128, 8], F32)
        self.dma(g1, I["gmix%d" % l])
        self.dma(g2, I["gffn%d" % l])
        self.stt("dve", self.A1, self.modT[:, 8:16], 1.0, g1, ALU.add, ALU.mult)
        self.stt("dve", self.A2, self.modT[:, 32:40], 1.0, g2, ALU.add, ALU.mult)
        self.dbg("modT%d" % l, self.modT)
        self.dbg("gt1_%d" % l, self.gt1)
        self.top = mark

    def norm_tile(self, xt, A, sh, dst, ps_bank):
        mark = self.top
        junk = self.sb([128, DM], BF16)
        ssq = self.sb([128, 1], F32)
        rstd = self.sb([128, 1], F32)
        xn = self.sb([128, DM], BF16)
        self.act(junk, xt, AF.Square, accum=ssq)
        self.act(rstd, ssq, AF.Sqrt, scale=float(1.0 / DM), bias=self.eps_ap[:, 0:1])
        self.recip(rstd, rstd)
        self.ts("dve", xn, xt, rstd[:, 0:1], ALU.mult)
        pT = self.bank(ps_bank, 1024, BF16)
        for k in range(8):
            self.tr(pT[:, k * 128:(k + 1) * 128], xn[:, k * 128:(k + 1) * 128], self.ident_b)
        tmp = self.sb([128, 8, 128], F32)
        self.tt("dve", tmp, pT.rearrange("p (k t) -> p k t", k=8), A[:, 0:8].unsqueeze(2).to_broadcast([128, 8, 128]), ALU.mult)
        self.tt("pool", dst, tmp, sh.unsqueeze(2).to_broadcast([128, 8, 128]), ALU.add)
        self.top = mark

    def stage_h(self, l, xsrc):
        I = self.inp
        self.hT = self.sb([128, 8, SEQ], BF16)
        mark = self.top
        xts = [self.sb([128, DM], F32) for _ in range(2)]
        for tt in range(16):
            xt = xts[tt % 2]
            self.dma(xt, xsrc[tt * 128:(tt + 1) * 128, :], q="sp" if tt % 2 == 0 else "pool")
            self.norm_tile(xt, self.A1, self.modT[:, 0:8], self.hT[:, :, tt * 128:(tt + 1) * 128], 5 + (tt % 2))
        self.top = mark
        if ("hT%d" % l) in self.debug:
            d = self.dram("dbg_hT%d" % l, [128, 8, SEQ], BF16)
            self.dma(d, self.hT)

    def load_blk(self, l, name, dst=None, q="pool"):
        if dst is None:
            dst = self.sb([128, 8, 128], BF16)
        self.dma(dst, self.inp["win%d" % l][BLK[name]], q=q)
        return dst

    def proj(self, ps, wb, tg, n=512):
        for k in range(8):
            self.mm(ps, wb[:, k, :], self.hT[:, k, tg * 512: tg * 512 + n], start=(k == 0), stop=(k == 7))

    def stage_mixA(self, l):
        I = self.inp
        mark = self.top
        acw = self.sb([128, 4, 3], F32)
        self.dma(acw, I["acw%d" % l])
        wc = [self.sb([128, 8, 128], BF16) for _ in range(2)]
        wx = [self.sb([128, 8, 128], BF16) for _ in range(2)]
        wb = [self.sb([128, 8, 128], BF16) for _ in range(2)]
        zbuf = self.sb([128, 2 + SEQ], F32)
        y = self.sb([128, SEQ], F32)
        am = [self.sb([128, SEQ], BF16) for _ in range(2)]
        tmpc = [self.sb([128, 512], F32) for _ in range(2)]
        self.ms("pool", zbuf[:, 0:2], 0.0)
        def aload(ch_):
            b_ = ch_ % 2
            self.load_blk(l, "a_c%d" % ch_, wc[b_])
            self.load_blk(l, "a_x%d" % ch_, wx[b_])
            self.load_blk(l, "a_b%d" % ch_, wb[b_])

        aload(0)
        for ch in range(4):
            b = ch % 2
            if ch + 1 < 4:
                aload(ch + 1)
            for tg in range(4):
                pc = self.bank(0 + 2 * (tg % 2))
                px = self.bank(1 + 2 * (tg % 2))
                self.proj(pc, wc[b], tg)
                self.proj(px, wx[b], tg)
                self.cp("act", tmpc[tg % 2], pc)
                self.tt("dve", zbuf[:, 2 + tg * 512: 2 + (tg + 1) * 512], tmpc[tg % 2], px, ALU.mult)
            self.ts("dve", y, zbuf[:, 0:SEQ], acw[:, ch, 0:1], ALU.mult)
            self.stt("dve", y, zbuf[:, 1:SEQ + 1], acw[:, ch, 1:2], y, ALU.mult, ALU.add)
            self.stt("dve", y, zbuf[:, 2:SEQ + 2], acw[:, ch, 2:3], y, ALU.mult, ALU.add)
            for tg in range(4):
                pb = self.bank(4 + (tg % 2))
                self.proj(pb, wb[b], tg)
                self.tt("dve", am[b][:, tg * 512:(tg + 1) * 512], y[:, tg * 512:(tg + 1) * 512], pb, ALU.mult)
            self.dma(self.amixT[:, ch, :], am[b])
        self.top = mark

    def stage_mixB(self, l):
        I = self.inp
        mark = self.top
        bcw = self.sb([128, 4, 31], F32)
        bcb = self.sb([128, 4], F32)
        blg = self.sb([128, 4], F32)
        blb = self.sb([128, 4], F32)
        self.dma(bcw, I["bcw%d" % l])
        self.dma(bcb, I["bcb%d" % l])
        self.dma(blg, I["blg%d" % l])
        self.dma(blb, I["blb%d" % l])
        wa = [self.sb([128, 8, 128], BF16) for _ in range(2)]
        wg = [self.sb([128, 8, 128], BF16) for _ in range(2)]
        ubuf = [self.sb([128, 30 + SEQ], F32) for _ in range(2)]
        yc = self.sb([128, 4, SEQ], F32)
        sg = [self.sb([128, 512], F32) for _ in range(2)]
        for ch in range(4):
            b = ch % 2
            eng = "dve"
            if ch == 0:
                self.load_blk(l, "b_a0", wa[0])
                self.load_blk(l, "b_g0", wg[0])
            if ch + 1 < 4:
                self.load_blk(l, "b_a%d" % (ch + 1), wa[(ch + 1) % 2])
                self.load_blk(l, "b_g%d" % (ch + 1), wg[(ch + 1) % 2])
            self.ms("pool", ubuf[b][:, 0:30], 0.0)
            for tg in range(4):
                pa = self.bank(0 + 2 * (tg % 2))
                pg = self.bank(1 + 2 * (tg % 2))
                self.proj(pa, wa[b], tg)
                self.proj(pg, wg[b], tg)
                self.act(sg[tg % 2], pg, AF.Sigmoid)
                self.tt("dve", ubuf[b][:, 30 + tg * 512: 30 + (tg + 1) * 512], sg[tg % 2], pa, ALU.mult)
            yv = yc[:, ch, :]
            self.ts(eng, yv, ubuf[b][:, 0:SEQ], bcw[:, ch, 0:1], ALU.mult, bcb[:, ch:ch + 1], ALU.add)
            for k in range(1, 31):
                self.stt(eng, yv, ubuf[b][:, k:k + SEQ], bcw[:, ch, k:k + 1], yv, ALU.mult, ALU.add)
        self.dbg("bconv%d" % l, yc.rearrange("p a b -> p (a b)"))
        bm = self.sb([128, 4, SEQ], BF16)
        sq = [self.sb([128, 512], F32) for _ in range(2)]
        mean = self.sb([128, 512], F32)
        rstd = self.sb([128, 512], F32)
        t1 = [self.sb([128, 512], F32) for _ in range(2)]
        for tg in range(4):
            sl = slice(tg * 512, (tg + 1) * 512)
            ps_s = self.bank(4)
            ps_q = self.bank(5)
            for ch in range(4):
                self.mm(ps_s, self.ones_f, yc[:, ch, sl], start=(ch == 0), stop=(ch == 3))
            for ch in range(4):
                self.act(sq[ch % 2], yc[:, ch, sl], AF.Square)
                self.mm(ps_q, self.ones_f, sq[ch % 2], start=(ch == 0), stop=(ch == 3))
            self.ts("dve", mean, ps_s, float(1.0 / 512), ALU.mult)
            self.tt("dve", rstd, mean, mean, ALU.mult)
            self.stt("dve", rstd, ps_q, float(1.0 / 512), rstd, ALU.mult, ALU.subtract)
            self.act(rstd, rstd, AF.Sqrt, bias=self.eps_ap[:, 0:1])
            self.recip(rstd, rstd)
            for ch in range(4):
                t = t1[ch % 2]
                self.tt("dve", t, yc[:, ch, sl], mean, ALU.subtract)
                self.tt("pool", t, t, rstd, ALU.mult)
                self.act(bm[:, ch, sl], t, AF.Silu, bias=blb[:, ch:ch + 1], scale=blg[:, ch:ch + 1])
        self.dma(self.bmixT, bm)
        self.top = mark


def stage_peer2(P, l, xsrc, xdst, final):
    I = P.inp
    mark = P.top
    NG = 8
    if "peer_1group" in P.debug:
        NG = 1
    skT = P.sb([128, 16, 128], F32)
    P.dma(skT, I["skT%d" % l])
    W_sb = P.sb([128, 256, 128], BF16)
    h2T = [P.sb([128, 8, 256], BF16) for _ in range(2)]
    I3T = [P.sb([128, 3, 256], BF16) for _ in range(2)]
    iota_b = P.sb([128, 128], BF16)
    P.cp("pool", iota_b, P.iota)
    xt2 = [P.sb([128, DM], F32) for _ in range(2)]
    uTb = [P.sb([128, 2, 8, 128], BF16) for _ in range(2)]
    vbb = [P.sb([128, 2, DM], BF16) for _ in range(2)]
    utv = P.utS.rearrange("c p k i -> p c k i")
    vbv2 = P.vbS.rearrange("c i d -> i c d")
    Gb = [P.sb([128, 256], BF16) for _ in range(2)]
    Wa = [P.sb([128, 256], BF16) for _ in range(3)]
    etmp = [P.sb([128, 512], F32) for _ in range(2)]
    if final:
        fng = P.sb([128, DM], F32)
        P.dma(fng, I["fng"].partition_broadcast(128), q="pool")
        ot = [P.sb([128, DM], F32) for _ in range(2)]
        fjunk = P.sb([128, DM], BF16)
        fssq = P.sb([128, 1], F32)
        frstd = P.sb([128, 1], F32)
    mT = P.top
    G6 = P.bank(6)
    G7 = P.bank(7)
    sc_sb = P.sb([128, 2, 16, 128], F32)
    mT2 = P.top
    xtp = [P.sb([128, DM], F32) for _ in range(2)]
    wqb = [P.sb([128, 8, 128], BF16) for _ in range(2)]
    qf = [P.sb([128, 256], F32) for _ in range(2)]
    sq = [P.sb([128, 256], F32) for _ in range(2)]
    rs = [P.sb([128, 256], F32) for _ in range(2)]
    qn = [P.sb([128, 256], F32) for _ in range(2)]
    norm_base = P.top
    P.top = mT2
    V = P.sb([128, 16, 16], F32)
    IDX = P.sb([128, 16, 16], U32)
    IDXf = P.sb([128, 16, 16], F32)
    tmpa = [P.sb([128, 128], F32) for _ in range(4)]
    cand = P.sb([128, 8, 256], F32)
    tmpc = [P.sb([128, 256], F32) for _ in range(4)]
    TOP = P.sb([128, 8, 16], F32)
    CI = P.sb([128, 8, 16], U32)
    AKu = P.sb([128, 8, 16], U32)
    BKu = P.sb([128, 8, 16], U32)
    AK = P.sb([128, 8, 16], F32)
    BK = P.sb([128, 8, 16], F32)
    oh = P.sb([128, 128, 16], F32)
    I1S = P.sb([128, 128], F32)
    I2S = P.sb([128, 128], F32)
    dsm = P.sb([128, 8, 16], F32)
    ssum = P.sb([128, 8], F32)
    Gm = P.sb([128, 8, 16], F32)
    pre_end = max(P.top, norm_base + 9 * 1024)
    iota16 = P.iota[:, 0:16].unsqueeze(1).unsqueeze(1).to_broadcast([128, 8, 16, 16])
    P.top = mT
    A = [P.sb([128, 16, 128], BF16) for _ in range(2)]
    B = [P.sb([128, 16, 128], BF16) for _ in range(2)]
    P.top = max(P.top, pre_end)
    P.maxtop = max(P.maxtop, P.top)
    assert P.top <= 207 * 1024, P.top

    def pre_slices(g):
        par = g % 2
        t0 = g * 256
        out = []

        def norm(t2):
            def f():
                old = P.top
                P.top = norm_base
                P.dma(xtp[t2], xsrc[t0 + t2 * 128: t0 + (t2 + 1) * 128, :])
                P.norm_tile(xtp[t2], P.A2, P.modT[:, 24:32], h2T[par][:, :, t2 * 128:(t2 + 1) * 128], 6 + t2)
                P.top = old
            return f

        out.append(norm(0))
        out.append(norm(1))

        def q_s1(j):
            b = j % 2
            P.dma(wqb[b], I["wq%d" % l][:, :, j * 128:(j + 1) * 128], q="pool")
            q_ps = P.bank(6)[:, 0:256]
            for k in range(8):
                P.mm(q_ps, wqb[b][:, k, :], h2T[par][:, k, :], start=(k == 0), stop=(k == 7), xw=[G6])
            P.cp("act", qf[b], q_ps, xr=[G6])
            P.act(sq[b], q_ps, AF.Square, xr=[G6])

        def q_s2(j):
            b = j % 2
            s_ps = P.bank(7)[:, 0:256]
            P.mm(s_ps, P.ones_f, sq[b], xw=[G7])
            P.ts("dve", rs[b], s_ps, float(1.0 / 128), ALU.mult, 1.0e-6, ALU.add, xr=[G7])
            P.act(rs[b], rs[b], AF.Sqrt)
            P.recip(rs[b], rs[b])
            P.tt("dve", qn[b], qf[b], rs[b], ALU.mult)
            c_ps = P.bank(7)[:, 256:512]
            for t2 in range(2):
                P.mm(c_ps[:, t2 * 128:(t2 + 1) * 128], qn[b][:, t2 * 128:(t2 + 1) * 128], skT[:, j, :], xw=[G7])
            P.cp("dve", sc_sb[:, :, j, :], c_ps.rearrange("p (a n) -> p a n", a=2), xr=[G7])

        out.append(lambda: q_s1(0))
        for j in range(16):
            def f(j=j):
                if j + 1 < 16:
                    q_s1(j + 1)
                q_s2(j)
            out.append(f)

        outA = out
        out = []
        for t2 in range(2):
            for j0 in range(0, 16, 4):
                out.append(lambda t2=t2, j0=j0: _top16_multi(P, [(sc_sb[:, t2, j, :], V[:, j, :], IDX[:, j, :], tmpa[j % 4]) for j in range(j0, j0 + 4)]))

            def f1(t2=t2):
                P.cp("dve", IDXf, IDX)
                V4 = V.rearrange("p (h two) a -> p h two a", two=2)
                P.tt("dve", cand.rearrange("p h (a b) -> p h a b", a=16),
                     V4[:, :, 0, :].unsqueeze(3).to_broadcast([128, 8, 16, 16]),
                     V4[:, :, 1, :].unsqueeze(2).to_broadcast([128, 8, 16, 16]), ALU.add)
            out.append(f1)
            for h0 in range(0, 8, 4):
                out.append(lambda h0=h0: _top16_multi(P, [(cand[:, h, :], TOP[:, h, :], CI[:, h, :], tmpc[h % 4]) for h in range(h0, h0 + 4)]))

            def f2(t2=t2):
                P.ts("dve", AKu, CI, 4, ALU.logical_shift_right)
                P.ts("dve", BKu, CI, 15, ALU.bitwise_and)
                P.cp("dve", AK, AKu)
                P.cp("dve", BK, BKu)
                IDX4 = IDXf.rearrange("p (h two) a -> p h two a", two=2)
                oh4 = oh.rearrange("p (h k) a -> p h k a", h=8)
                for XK, pidx, OUT in ((AK, 0, I1S), (BK, 1, I2S)):
                    P.tt("dve", oh4, iota16, XK.unsqueeze(3).to_broadcast([128, 8, 16, 16]), ALU.is_equal)
                    P.tt("dve", oh4, oh4, IDX4[:, :, pidx, :].unsqueeze(2).to_broadcast([128, 8, 16, 16]), ALU.mult)
                    P.red("dve", OUT, oh, ALU.add)
            out.append(f2)

            def f3(t2=t2):
                P.tt("dve", dsm, TOP, TOP[:, :, 0:1].to_broadcast([128, 8, 16]), ALU.subtract)
                P.act(dsm, dsm, AF.Exp)
                P.red("dve", ssum, dsm, ALU.add)
                P.recip(ssum, ssum)
                P.tt("dve", Gm, dsm, ssum.unsqueeze(2).to_broadcast([128, 8, 16]), ALU.mult)
                tr_ps = P.bank(6)[:, 0:384]
                P.tr(tr_ps[:, 0:128], I1S, P.ident_f, xw=[G6])
                P.tr(tr_ps[:, 128:256], I2S, P.ident_f, xw=[G6])
                P.tr(tr_ps[:, 256:384], Gm.rearrange("p h k -> p (h k)"), P.ident_f, xw=[G6])
                P.cp("act", I3T[par][:, :, t2 * 128:(t2 + 1) * 128], tr_ps.rearrange("p (a t) -> p a t", a=3), xr=[G6])
            out.append(f3)
        return outA, out

    def w_build(g):
        par = g % 2

        def ab_build(sbi):
            ab = sbi % 2
            tsl = slice(sbi * 16, sbi * 16 + 16)
            P.tt("dve", B[ab], iota_b.unsqueeze(1).to_broadcast([128, 16, 128]),
                 I3T[par][:, 1, tsl].unsqueeze(2).to_broadcast([128, 16, 128]), ALU.is_equal)
            for tl in range(16):
                t = sbi * 16 + tl
                P.ts("dve", A[ab][:, tl, :], iota_b, I3T[par][:, 0, t:t + 1], ALU.is_equal, I3T[par][:, 2, t:t + 1], ALU.mult)

        ab_build(0)
        for sbi in range(16):
            ab = sbi % 2
            for q4 in range(4):
                w_ps = P.bank(q4)
                for u in range(4):
                    tl = q4 * 4 + u
                    P.mm(w_ps[:, u * 128:(u + 1) * 128], B[ab][:, tl, :], A[ab][:, tl, :])
            if sbi + 1 < 16:
                ab_build(sbi + 1)
            for q4 in range(4):
                w_ps = P.bank(q4)
                tb = sbi * 16 + q4 * 4
                P.cp("act", W_sb[:, tb:tb + 4, :], w_ps.rearrange("p (t c) -> p t c", t=4))

    def main_loop(g, slices):
        par = g % 2

        def s_mm(c):
            if c % 2 == 0:
                P.dma(uTb[(c // 2) % 2], utv[:, c:c + 2])
                P.dma(vbb[(c // 2) % 2], vbv2[:, c:c + 2, :])
            ub_ = uTb[(c // 2) % 2][:, c % 2]
            S_ps = P.bank(4 + c % 2)[:, 0:256]
            for k in range(8):
                P.mm(S_ps, ub_[:, k, :], h2T[par][:, k, :], start=(k == 0), stop=(k == 7))

        ns = len(slices)
        done = 0
        s_mm(0)
        for c in range(128):
            if c + 1 < 128:
                s_mm(c + 1)
            vb_ = vbb[(c // 2) % 2][:, c % 2]
            S_ps = P.bank(4 + c % 2)[:, 0:256]
            P.act(Gb[c % 2], S_ps, AF.Gelu)
            wa = Wa[c % 3]
            P.tt("dve" if c % 2 == 0 else "pool", wa, Gb[c % 2], W_sb[:, :, c], ALU.mult)
            for t2 in range(2):
                for half in range(2):
                    P.mm(P.bank(t2 * 2 + half), wa[:, t2 * 128:(t2 + 1) * 128], vb_[:, half * 512:(half + 1) * 512],
                         start=(c == 0), stop=(c == 127))
            want = min(ns, ((c + 1) * ns + 111) // 112)
            while done < want:
                slices[done]()
                done += 1
        while done < ns:
            slices[done]()
            done += 1

    def epilogue(g):
        t0 = g * 256
        for t2 in range(2):
            x_ = xt2[t2]
            r0 = t0 + t2 * 128
            P.dma(x_, xsrc[r0:r0 + 128, :])
            for half in range(2):
                hs = slice(half * 512, (half + 1) * 512)
                e_ = etmp[half]
                P.tt("dve", e_, P.gt2[:, hs], P.bank(t2 * 2 + half), ALU.mult)
                P.tt("pool", x_[:, hs], x_[:, hs], e_, ALU.add)
            if not final:
                P.dma(xdst[r0:r0 + 128, :], x_)
            else:
                P.act(fjunk, x_, AF.Square, accum=fssq)
                P.act(frstd, fssq, AF.Sqrt, scale=float(1.0 / DM), bias=P.eps_ap[:, 0:1])
                P.recip(frstd, frstd)
                o_ = ot[t2]
                P.ts("dve", o_, x_, frstd[:, 0:1], ALU.mult)
                P.tt("pool", o_, o_, fng, ALU.mult)
                P.dma(xdst[r0:r0 + 128, :], o_)

    pa, pb = pre_slices(0)
    for f in pa + pb:
        f()
    for g in range(NG):
        w_build(g)
        pb = []
        if g + 1 < NG:
            pa, pb = pre_slices(g + 1)
            for f in pa:
                f()
        main_loop(g, pb)
        epilogue(g)
    P.top = mark


def input_shapes(shared, core):
    shapes = {}
    for k, v in list(shared.items()) + list(core.items()):
        dt = {np.dtype(np.float32): F32, np.dtype(np.int32): I32, np.dtype(NPBF): BF16}[v.dtype]
        shapes[k] = (v.shape, dt)
    return shapes


def build(shapes, upto="all", debug=()):
    P = Prog(shapes, debug)
    I = P.inp
    P.load_consts()
    P.amixT = P.dram("s_amixT", [128, 4, SEQ], BF16)
    P.bmixT = P.dram("s_bmixT", [128, 4, SEQ], BF16)
    P.ocT = P.dram("s_ocT", [8, 64, SEQ], BF16)
    P.odT = P.dram("s_odT", [8, 64, SEQ], BF16)
    if upto in ("peer", "peer_only", "all"):
        P.utS = P.dram("s_ut", [128, 128, 8, 128], BF16)
        P.vbS = P.dram("s_vb", [128, 128, DM], BF16)
    xsrc = I["xin"]
    for l in range(DEPTH):
        base = P.top
        P.stage_mod(l)
        xmid = P.dram("s_xmid%d" % l, [SEQ, DM], F32)
        if upto == "peer_only":
            stage_peer_prep(P, l)
            xo = P.dram("s_xout%d" % l, [SEQ, DM], F32)
            stage_peer2(P, l, I["xmid_in"], xo, False)
            break
        hmark = P.top
        P.stage_h(l, xsrc)
        if upto not in ("nsa_only", "moba_only"):
            P.stage_mixA(l)
        if upto == "mixA":
            break
        if upto not in ("nsa_only", "moba_only"):
            P.stage_mixB(l)
        if upto == "mixB":
            break
        if upto != "moba_only":
            stage_nsa(P, l)
        if upto in ("nsa", "nsa_only"):
            break
        stage_moba(P, l)
        if upto in ("moba", "moba_only"):
            break
        stage_merge(P, l, xsrc, xmid)
        if upto == "merge":
            break
        P.top = hmark
        stage_peer_prep(P, l)
        if l == DEPTH - 1:
            xo = P.dram("out", [SEQ, DM], F32)
        else:
            xo = P.dram("s_xout%d" % l, [SEQ, DM], F32)
        stage_peer2(P, l, xmid, xo, l == DEPTH - 1)
        if upto == "peer":
            break
        xsrc = xo
        P.top = base
    P.stats = P.S.finalize()
    return P


def core_inputs(inp, b):
    return dict(
        xin=np.ascontiguousarray(inp["x"][b]),
        cT=np.ascontiguousarray(inp["c"][b].reshape(8, 128).T),
        pos=np.ascontiguousarray(inp["positions"][b][None, :].astype(np.int32)),
    )


def _rope_evac(P, p1, p2, rows, dst, tg, i):
    sl = slice(tg * 512, (tg + 1) * 512)
    t1 = P.rt1[i % 2]
    t2 = P.rt2[i % 2]
    P.tt("dve", t1, P.cos64[:, sl], p1[rows], ALU.mult)
    P.tt("dve", t2, P.sinS[:, sl], p2[rows], ALU.mult)
    P.tt("pool", dst, t1, t2, ALU.add)


SBANKS = (0, 1, 6)


def _run_tiles(P, tiles, look=2):
    n = len(tiles)
    slot = [None] * n

    def qk(i):
        j = P.tile_ctr
        P.tile_ctr += 1
        slot[i] = j
        P.mm(P.bank(SBANKS[j % 3]), tiles[i]["kT"], tiles[i]["qT"])

    for i in range(min(look, n)):
        qk(i)
    for i in range(n):
        if i + look < n:
            qk(i + look)
        t = tiles[i]
        j = slot[i]
        s_ps = P.bank(SBANKS[j % 3])
        E = P.Ebuf[j % 3]
        P.act(E, s_ps, AF.Exp, scale=0.125)
        if t["mask"] is not None:
            P.tt("dve" if j % 2 == 0 else "pool", E, E, t["mask"], ALU.mult)
        P.mm(t["num"], t["vT"], E, start=t["first"], stop=t["last"])
        P.mm(t["den"], P.ones_b, E, start=t["first"], stop=t["last"])
        if t["post"] is not None:
            t["post"]()


def _grep(P, br, h, tg, i):
    g_ps = P.bank(7)[0:64]
    P.mm(g_ps, P.selall[:, br * 8 + h, :], P.GS[:, tg * 512:(tg + 1) * 512])
    gr = P.grbuf[i % 2]
    P.cp("act", gr, g_ps)
    return gr


def stage_nsa(P, l):
    I = P.inp
    mark = P.top
    P.load_attn_consts()
    QA = P.sb([128, 8, SEQ], BF16)
    KC = P.sb([64, 2, SEQ + 16], BF16)
    VCt = P.sb([64, 2, SEQ + 16], BF16)
    KS = P.sb([128, 2, SEQ], BF16)
    KW = P.sb([64, 2, SEQ], BF16)
    VS = P.sb([128, 16, 128], BF16)
    VW = P.sb([128, 16, 128], BF16)
    P.GS = P.sb([32, SEQ], BF16)
    KCMP = P.sb([64, 2, 128], BF16)
    VCMP = P.sb([128, 2, 64], BF16)
    mA = P.top
    P.rt1 = [P.sb([64, 512], F32) for _ in range(2)]
    P.rt2 = [P.sb([64, 512], F32) for _ in range(2)]
    wbl = [P.sb([128, 8, 128], BF16) for _ in range(4)]
    cnt = 0
    jobs = [("c_q%d" % j, "c_q_sw%d" % j, QA, 2 * j, 2 * j + 1) for j in range(4)]
    jobs += [(nm, nm + "_sw", dstT, 0, 1) for nm, dstT in (("c_kc", KC), ("c_ks", KS), ("c_kw", KW))]

    def jload(p):
        return (P.load_blk(l, jobs[p][0], wbl[2 * (p % 2)]), P.load_blk(l, jobs[p][1], wbl[2 * (p % 2) + 1]))

    wcur = jload(0)
    for p in range(len(jobs)):
        wnext = jload(p + 1) if p + 1 < len(jobs) else None
        w1, w2 = wcur
        dstT, ha, hb = jobs[p][2], jobs[p][3], jobs[p][4]
        for tg in range(4):
            p1 = P.bank(0 + 2 * (cnt % 2))
            p2 = P.bank(1 + 2 * (cnt % 2))
            P.proj(p1, w1, tg)
            P.proj(p2, w2, tg)
            sl = slice(tg * 512, (tg + 1) * 512)
            _rope_evac(P, p1, p2, slice(0, 64), dstT[0:64, ha, sl], tg, cnt)
            _rope_evac(P, p1, p2, slice(64, 128), dstT[0:64, hb, sl], tg, cnt + 1)
            cnt += 1
        wcur = wnext
    w1 = P.load_blk(l, "c_vc", wbl[2])
    for tg in range(4):
        p1 = P.bank(4 + (tg % 2))
        P.proj(p1, w1, tg)
        sl = slice(tg * 512, (tg + 1) * 512)
        P.cp("act", VCt[:, 0, sl], p1[0:64])
        P.cp("dve", VCt[:, 1, sl], p1[64:128])
    w1 = P.load_blk(l, "c_gate", wbl[3])
    for tg in range(4):
        p1 = P.bank(6 + (tg % 2))
        P.proj(p1, w1, tg)
        P.act(P.GS[:, tg * 512:(tg + 1) * 512], p1[0:32], AF.Sigmoid)
    for nm, dstV in (("c_vs", VS), ("c_vw", VW)):
        w1 = P.load_blk(l, nm, wbl[0] if nm == "c_vs" else wbl[1])
        for t16 in range(16):
            ps = P.bank(4 + (t16 % 2))[:, 0:128]
            for k in range(8):
                P.mm(ps, P.hT[:, k, t16 * 128:(t16 + 1) * 128], w1[:, k, :], start=(k == 0), stop=(k == 7))
            P.cp("act" if t16 % 2 == 0 else "dve", dstV[:, t16, :], ps)
    P.ms("pool", KS[64:128], 0.0)
    P.ms("pool", QA[64:128], 0.0)
    for g in range(2):
        P.dma(KS[64:96, g, :], I["k_slcaug"])
    if "stop_proj" in P.debug:
        P.top = mark
        return
    P.top = mA
    w1c = P.sb([64, 2, 32, 128], BF16)
    P.dma(w1c, I["w1r%d" % l], q="pool")
    w2c = P.sb([128, 2, 64], BF16)
    P.dma(w2c, I["w2r%d" % l], q="pool")
    posf = P.sb([64, 2, 32], F32)
    P.dma(posf, I["posT%d" % l])
    hid = [P.sb([128, 128], BF16) for _ in range(2)]
    srcA = [P.sb([64, SEQ], BF16) for _ in range(2)]
    srcB = [P.sb([64, SEQ], BF16) for _ in range(2)]
    for g in range(2):
        P.ms("pool", KC[:, g, SEQ:SEQ + 16], 0.0)
        P.ms("pool", VCt[:, g, SEQ:SEQ + 16], 0.0)
    ci = 0
    for kv in range(2):
        SRC = KC if kv == 0 else VCt
        for g in range(2):
            sa = srcA[ci % 2]
            sbb = srcB[ci % 2]
            ci += 1
            P.tt("dve", sa.rearrange("p (n s) -> p n s", s=16), SRC[:, g, 0:SEQ].rearrange("p (n s) -> p n s", s=16),
                 posf[:, kv, 0:16].unsqueeze(1).to_broadcast([64, 128, 16]), ALU.add)
            P.tt("dve", sbb.rearrange("p (n s) -> p n s", s=16), SRC[:, g, 16:SEQ + 16].rearrange("p (n s) -> p n s", s=16),
                 posf[:, kv, 16:32].unsqueeze(1).to_broadcast([64, 128, 16]), ALU.add)
            sa3 = sa.rearrange("p (n s) -> p n s", s=16)
            sb3 = sbb.rearrange("p (n s) -> p n s", s=16)
            h_ps = P.bank(7)[:, 0:128]
            for li in range(32):
                rhs = sa3[:, :, li] if li < 16 else sb3[:, :, li - 16]
                P.mm(h_ps, w1c[:, kv, li, :], rhs, start=(li == 0), stop=(li == 31))
            hd = hid[g]
            P.act(hd, h_ps, AF.Gelu)
            if kv == 0:
                kc_ps = P.bank(6)[0:64, 128:256]
                P.mm(kc_ps, w2c[:, 0, :], hd)
                P.cp("act", KCMP[:, g, :], kc_ps)
            else:
                vc_ps = P.bank(6)[:, 256:320]
                P.mm(vc_ps, hd, w2c[:, 1, :])
                P.cp("act", VCMP[:, g, :], vc_ps)
    if "stop_cmp" in P.debug:
        P.top = mark
        return
    P.top = mA
    cmpmask = P.sb([128, SEQ], BF16)
    P.dma(cmpmask, I["k_cmpmask"])
    overlap = P.sb([128, 32], F32)
    P.dma(overlap, I["k_overlap"])
    slcadd = P.sb([128, 16, 32], F32)
    P.dma(slcadd, I["k_slcadd"])
    P.selall = P.sb([32, 24, 64], BF16)
    P.dma(P.selall, I["k_selall"])
    SBm = P.sb([128, 4, 2, 128], BF16)
    P.ms("pool", SBm, 0.0)
    OC = P.sb([64, 8, 512], F32)
    P.Ebuf = [P.sb([128, 512], BF16) for _ in range(3)]
    P.grbuf = [P.sb([64, 512], F32) for _ in range(2)]
    Ef = [P.sb([128, 512], F32) for _ in range(2)]
    rdf = [P.sb([128, 512], F32) for _ in range(2)]
    Pn = [P.sb([128, 512], F32) for _ in range(2)]
    Pb = [P.sb([128, 512], BF16) for _ in range(2)]
    vsel = [P.sb([128, 32], F32) for _ in range(2)]
    psum_g = P.sb([128, 512], F32)
    vtmp = [P.sb([128, 32], F32) for _ in range(2)]
    m8a = [P.sb([128, 8], F32) for _ in range(2)]
    m8b = [P.sb([128, 8], F32) for _ in range(2)]
    rd64 = [P.sb([64, 512], F32) for _ in range(2)]
    ctmp = [P.sb([64, 512], F32) for _ in range(2)]
    ocb = [P.sb([64, 512], BF16) for _ in range(2)]
    dbg_br = {}
    for nm in ("cmp", "slc", "win"):
        if ("ob_%s" % nm) in P.debug:
            dbg_br[nm] = P.dram("dbg_ob_%s" % nm, [8, 64, SEQ], F32)
    P.tile_ctr = 0
    gi = 0
    ei = 0
    for tg in range(4):
        sl = slice(tg * 512, (tg + 1) * 512)
        imp4 = P.bank(6)[:, 0:256].rearrange("p (a b c) -> p a b c", a=4, b=2)
        for h in range(8):
            g, r = h // 4, h % 4
            s_ps = P.bank(h % 2)
            P.mm(s_ps, KCMP[:, g, :], QA[0:64, h, sl])
            E = Ef[h % 2]
            P.act(E, s_ps, AF.Exp, scale=0.125)
            P.tt("dve", E, E, cmpmask[:, sl], ALU.mult)
            d_ps = P.bank(2 + h % 2)
            P.mm(d_ps, P.ones_f, E)
            rd = rdf[h % 2]
            P.ts("dve", rd, d_ps, 1.0e-18, ALU.max)
            P.recip_act(rd, rd)
            pn = Pn[h % 2]
            P.tt("dve", pn, E, rd, ALU.mult)
            pb = Pb[h % 2]
            P.cp("pool", pb, pn)
            o_ps = P.bank(4 + h % 2)[0:64]
            P.mm(o_ps, VCMP[:, g, :], pb)
            if r == 0:
                P.cp("pool", psum_g, pn)
            else:
                P.tt("pool", psum_g, psum_g, pn, ALU.add)
            if r == 3:
                for t4 in range(4):
                    P.mm(imp4[:, t4, g, :], psum_g[:, t4 * 128:(t4 + 1) * 128], overlap)
            gr = _grep(P, 0, h, tg, gi)
            gi += 1
            P.tt("dve", OC[:, h, :], gr, o_ps, ALU.mult)
            if "cmp" in dbg_br:
                P.cp("dve", ctmp[0], o_ps)
                P.dma(dbg_br["cmp"][h, :, sl], ctmp[0])
        if "imp" in P.debug and tg == 0:
            impd = P.dram("dbg_imp", [128, 256], F32)
            impsb = P.sb([128, 256], F32)
            P.cp("dve", impsb, P.bank(6)[:, 0:256])
            P.dma(impd, impsb)
        if "stop_c1" in P.debug:
            break
        for t4 in range(4):
            for g in range(2):
                i2 = (t4 * 2 + g) % 2
                v = vsel[i2]
                P.tt("dve", v, slcadd[:, tg * 4 + t4, :], imp4[:, t4, g, :], ALU.add)
                P.S.emit("dve", lambda e, o=m8a[i2], i=v: e.max(out=o, in_=i), w=[m8a[i2]], r=[v])
                P.S.emit("dve", lambda e, o=vtmp[i2], a=m8a[i2], b=v: e.match_replace(out=o, in_to_replace=a, in_values=b, imm_value=-3.0e38), w=[vtmp[i2]], r=[m8a[i2], v])
                P.S.emit("dve", lambda e, o=m8b[i2], i=vtmp[i2]: e.max(out=o, in_=i), w=[m8b[i2]], r=[vtmp[i2]])
                P.ts("dve", SBm[:, t4, g, 64:96], v, m8b[i2][:, 7:8], ALU.is_ge, 1.0, ALU.subtract)
                bt = P.bank(0 + g)[:, t4 * 128:(t4 + 1) * 128]
                P.mm(bt, SBm[:, t4, g, :], P.ident_b)
        for g in range(2):
            for r in range(4):
                P.cp("act" if g == 0 else "dve", QA[64:96, 4 * g + r, sl], P.bank(0 + g)[64:96, :])
        if "stop_s1" in P.debug:
            break
        tiles = []
        for h in range(8):
            g = h // 4
            for bi, nm in ((1, "slc"), (2, "win")):
                num = P.bank(2 + 2 * (ei % 2))[0:64]
                den = P.bank(3 + 2 * (ei % 2))[0:64]
                if nm == "slc":
                    kts = list(range(0, 4 * tg + 4))
                else:
                    kts = list(range(max(0, 4 * tg - 4), 4 * tg + 4))

                def post(h=h, bi=bi, nm=nm, num=num, den=den, e=ei, sl=sl, tg=tg):
                    rd = rd64[e % 2]
                    P.ts("dve", rd, den, 1.0e-18, ALU.max)
                    P.recip_act(rd, rd)
                    if nm in dbg_br:
                        P.tt("dve", ctmp[e % 2], rd, num, ALU.mult)
                        P.dma(dbg_br[nm][h, :, sl], ctmp[e % 2])
                    gr = _grep(P, bi, h, tg, e)
                    P.tt("dve", rd, rd, gr, ALU.mult)
                    P.tt("dve", ctmp[e % 2], rd, num, ALU.mult)
                    P.tt("pool", OC[:, h, :], OC[:, h, :], ctmp[e % 2], ALU.add)
                    if nm == "win":
                        ob = ocb[h % 2]
                        P.cp("act", ob, OC[:, h, :])
                        P.dma(P.ocT[h, :, sl], ob)

                for ii, kt in enumerate(kts):
                    ks = slice(kt * 128, (kt + 1) * 128)
                    if kt >= 4 * tg:
                        mask = P.cmask[:, kt - 4 * tg, :]
                    elif nm == "win":
                        mask = P.wmask[:, kt - (4 * tg - 4), :]
                    else:
                        mask = None
                    last = ii == len(kts) - 1
                    if nm == "slc":
                        tiles.append(dict(kT=KS[:, g, ks], qT=QA[:, h, sl], vT=VS[:, kt, g * 64:(g + 1) * 64], mask=mask,
                                          num=num, den=den, first=ii == 0, last=last, post=post if last else None))
                    else:
                        tiles.append(dict(kT=KW[0:64, g, ks], qT=QA[0:64, h, sl], vT=VW[:, kt, g * 64:(g + 1) * 64], mask=mask,
                                          num=num, den=den, first=ii == 0, last=last, post=post if last else None))
                ei += 1
        _run_tiles(P, tiles)
    P.top = mark


def stage_moba(P, l):
    I = P.inp
    mark = P.top
    P.load_attn_consts()
    QB = P.sb([128, 8, SEQ], BF16)
    KB = P.sb([128, 8, SEQ], BF16)
    VB = P.sb([128, 16, 512], BF16)
    P.ms("pool", QB[64:128], 0.0)
    P.ms("pool", KB[64:128], 0.0)
    for h in range(8):
        P.dma(KB[64:72, h, :], I["k_mobaug"])
    mA = P.top
    P.rt1 = [P.sb([64, 512], F32) for _ in range(2)]
    P.rt2 = [P.sb([64, 512], F32) for _ in range(2)]
    wbl = [P.sb([128, 8, 128], BF16) for _ in range(4)]
    cnt = 0
    jobs = [("%s%d" % (nm, j), "%s_sw%d" % (nm, j), dstT, 2 * j, 2 * j + 1) for nm, dstT in (("d_q", QB), ("d_k", KB)) for j in range(4)]

    def jload(p):
        return (P.load_blk(l, jobs[p][0], wbl[2 * (p % 2)]), P.load_blk(l, jobs[p][1], wbl[2 * (p % 2) + 1]))

    wcur = jload(0)
    for p in range(len(jobs)):
        wnext = jload(p + 1) if p + 1 < len(jobs) else None
        w1, w2 = wcur
        dstT, ha, hb = jobs[p][2], jobs[p][3], jobs[p][4]
        for tg in range(4):
            p1 = P.bank(0 + 2 * (cnt % 2))
            p2 = P.bank(1 + 2 * (cnt % 2))
            P.proj(p1, w1, tg)
            P.proj(p2, w2, tg)
            sl = slice(tg * 512, (tg + 1) * 512)
            _rope_evac(P, p1, p2, slice(0, 64), dstT[0:64, ha, sl], tg, cnt)
            _rope_evac(P, p1, p2, slice(64, 128), dstT[0:64, hb, sl], tg, cnt + 1)
            cnt += 1
        wcur = wnext
    wv = P.sb([128, 8, 512], BF16)
    wv4 = wv.rearrange("p k (j c) -> p k j c", j=4)
    for j in range(4):
        P.dma(wv4[:, :, j, :], I["win%d" % l][BLK["d_v%d" % j]], q="pool")
    for t16 in range(16):
        ps = P.bank(4 + (t16 % 2))
        for k in range(8):
            P.mm(ps, P.hT[:, k, t16 * 128:(t16 + 1) * 128], wv[:, k, :], start=(k == 0), stop=(k == 7))
        P.cp("act" if t16 % 2 == 0 else "dve", VB[:, t16, :], ps)
    if "stop_mproj" in P.debug:
        P.top = mark
        return
    P.top = mA
    km = P.sb([64, 8, 8], F32)
    kmb = P.sb([64, 8, 32], BF16)
    P.ms("pool", kmb, 0.0)
    for h in range(8):
        P.red("dve", km[:, h, :], KB[0:64, h, :].rearrange("p (n s) -> p n s", s=256), ALU.add)
    P.ts("dve", kmb[:, :, 0:8], km, float(1.0 / 256), ALU.mult)
    mobadd = P.sb([128, 8, 8], F32)
    P.dma(mobadd, I["k_mobadd"])
    notown = P.sb([128, 8, 8], F32)
    P.dma(notown, I["k_notown"])
    SBq = [P.sb([128, 8, 128], BF16) for _ in range(2)]
    P.ms("pool", SBq[0], 0.0)
    P.ms("pool", SBq[1], 0.0)
    vv = [P.sb([128, 8, 8], F32) for _ in range(2)]
    M8 = [P.sb([128, 8, 8], F32) for _ in range(2)]
    cc = [P.sb([128, 8, 8], F32) for _ in range(2)]
    for t16 in range(8, 16):
        own = t16 // 2
        b = t16 % 2
        ts_ = slice(t16 * 128, (t16 + 1) * 128)
        g_ps = P.bank(6 + b)[:, 0:256].rearrange("p (h n) -> p h n", h=8)
        for h in range(8):
            P.mm(g_ps[:, h, :], QB[0:64, h, ts_], kmb[:, h, :])
        v = vv[b]
        P.tt("dve", v, g_ps[:, :, 0:8], mobadd[:, own, :].unsqueeze(1).to_broadcast([128, 8, 8]), ALU.add)
        for h in range(8):
            P.S.emit("dve", lambda e, o=M8[b][:, h, :], i=v[:, h, :]: e.max(out=o, in_=i), w=[M8[b][:, h, :]], r=[v[:, h, :]])
        P.tt("dve", cc[b], v, M8[b][:, :, 2:3].to_broadcast([128, 8, 8]), ALU.is_ge)
        P.ts("dve", cc[b], cc[b], 1.0, ALU.subtract)
        P.tt("dve", SBq[b][:, :, 64:72], cc[b], notown[:, own, :].unsqueeze(1).to_broadcast([128, 8, 8]), ALU.mult)
        for h in range(8):
            bt = P.bank(0 + (h // 4))[:, (h % 4) * 128:(h % 4 + 1) * 128]
            P.mm(bt, SBq[b][:, h, :], P.ident_b)
        for hb in range(2):
            src = P.bank(hb)[64:72, :].rearrange("p (h t) -> p h t", h=4)
            P.cp("act" if hb == 0 else "dve", QB[64:72, 4 * hb:4 * hb + 4, ts_], src)
    if "stop_mgate" in P.debug:
        P.top = mark
        return
    P.Ebuf = [P.sb([128, 512], BF16) for _ in range(3)]
    rd64 = [P.sb([64, 512], F32) for _ in range(2)]
    odb = [P.sb([64, 512], BF16) for _ in range(2)]
    P.tile_ctr = 0
    ei = 0
    for tg in range(4):
        sl = slice(tg * 512, (tg + 1) * 512)
        tiles = []
        for h in range(8):
            num = P.bank(2 + 2 * (ei % 2))[0:64]
            den = P.bank(3 + 2 * (ei % 2))[0:64]
            kts = list(range(0, 4 * tg + 4))

            def post(h=h, num=num, den=den, e=ei, sl=sl):
                rd = rd64[e % 2]
                P.ts("dve", rd, den, 1.0e-18, ALU.max)
                P.recip_act(rd, rd)
                P.tt("dve", odb[e % 2], rd, num, ALU.mult)
                P.dma(P.odT[h, :, sl], odb[e % 2])

            for ii, kt in enumerate(kts):
                ks = slice(kt * 128, (kt + 1) * 128)
                mask = P.cmask[:, kt - 4 * tg, :] if kt >= 4 * tg else None
                last = ii == len(kts) - 1
                tiles.append(dict(kT=KB[:, h, ks], qT=QB[:, h, sl], vT=VB[:, kt, h * 64:(h + 1) * 64], mask=mask,
                                  num=num, den=den, first=ii == 0, last=last, post=post if last else None))
            ei += 1
        _run_tiles(P, tiles)
    P.top = mark


def stage_merge(P, l, xsrc, xdst):
    I = P.inp
    mark = P.top
    WO = P.sb([128, 8, DM], BF16)
    wst = [P.sb([128, DM], F32) for _ in range(2)]
    for k in range(8):
        P.dma(wst[k % 2], I["wo%d" % l][:, k, :])
        P.tt("dve" if k % 2 == 0 else "pool", WO[:, k, :], wst[k % 2], P.gt1, ALU.mult)
    AM = P.sb([128, 4, 512], BF16)
    BM = P.sb([128, 4, 512], BF16)
    OCt = P.sb([64, 8, 512], BF16)
    ODt = P.sb([64, 8, 512], BF16)
    MG = P.sb([128, 8, 512], BF16)
    gw = [[P.sb([128, 8, 128], BF16) for _ in range(4)] for _ in range(2)]
    wa = [P.sb([128, 4, 128], BF16) for _ in range(2)]
    wb = [P.sb([128, 4, 128], BF16) for _ in range(2)]
    wc = [P.sb([64, 8, 128], BF16) for _ in range(2)]
    wd = [P.sb([64, 8, 128], BF16) for _ in range(2)]
    sg = [P.sb([128, 512], F32) for _ in range(2)]
    acc = [P.sb([128, 512], F32) for _ in range(2)]
    tmp = [P.sb([128, 512], F32) for _ in range(2)]
    xt = [P.sb([128, DM], F32) for _ in range(2)]
    ocv = P.ocT.rearrange("h p t -> p h t")
    odv = P.odT.rearrange("h p t -> p h t")
    it = 0
    for tg in range(4):
        sl = slice(tg * 512, (tg + 1) * 512)
        P.dma(AM, P.amixT[:, :, sl])
        P.dma(BM, P.bmixT[:, :, sl])
        P.dma(OCt, ocv[:, :, sl])
        P.dma(ODt, odv[:, :, sl])
        def wload(m_, b_):
            for br_ in range(4):
                P.load_blk(l, "merge%d" % (br_ * 8 + m_), gw[b_][br_])
            P.dma(wa[b_], I["a_out%d" % l][m_], q="pool")
            P.dma(wb[b_], I["b_out%d" % l][m_], q="pool")
            P.dma(wc[b_], I["c_out%d" % l][m_], q="pool")
            P.dma(wd[b_], I["d_out%d" % l][m_], q="pool")

        if tg == 0:
            wload(0, it % 2)
        for m in range(8):
            b = it % 2
            it += 1
            if m + 1 < 8:
                wload(m + 1, it % 2)
            elif tg + 1 < 4:
                wload(0, it % 2)
            for br in range(4):
                g_ps = P.bank(2 * (br % 2))
                y_ps = P.bank(2 * (br % 2) + 1)
                P.proj(g_ps, gw[b][br], tg)
                P.act(sg[br % 2], g_ps, AF.Sigmoid)
                if br == 0:
                    for k in range(4):
                        P.mm(y_ps, wa[b][:, k, :], AM[:, k, :], start=(k == 0), stop=(k == 3))
                elif br == 1:
                    for k in range(4):
                        P.mm(y_ps, wb[b][:, k, :], BM[:, k, :], start=(k == 0), stop=(k == 3))
                elif br == 2:
                    for h in range(8):
                        P.mm(y_ps, wc[b][:, h, :], OCt[:, h, :], start=(h == 0), stop=(h == 7))
                else:
                    for h in range(8):
                        P.mm(y_ps, wd[b][:, h, :], ODt[:, h, :], start=(h == 0), stop=(h == 7))
                if br == 0:
                    P.tt("dve", acc[b], sg[br % 2], y_ps, ALU.mult)
                else:
                    P.tt("dve", tmp[br % 2], sg[br % 2], y_ps, ALU.mult)
                    P.tt("pool", acc[b], acc[b], tmp[br % 2], ALU.add)
            P.cp("act", MG[:, m, :], acc[b])
        for t4 in range(4):
            x_ = xt[t4 % 2]
            r0 = tg * 512 + t4 * 128
            P.dma(x_, xsrc[r0:r0 + 128, :])
            for half in range(2):
                o_ps = P.bank(4 + half)
                for k in range(8):
                    P.mm(o_ps, MG[:, k, t4 * 128:(t4 + 1) * 128], WO[:, k, half * 512:(half + 1) * 512], start=(k == 0), stop=(k == 7))
                P.tt("dve", x_[:, half * 512:(half + 1) * 512], x_[:, half * 512:(half + 1) * 512], o_ps, ALU.add)
            P.dma(xdst[r0:r0 + 128, :], x_)
    P.top = mark


def _emit_max(P, out, in_):
    P.S.emit("dve", lambda e: e.max(out=out, in_=in_), w=[out], r=[in_])


def _emit_maxidx(P, out, mx, vals):
    P.S.emit("dve", lambda e: e.max_index(out=out, in_max=mx, in_values=vals), w=[out], r=[mx, vals])


def _emit_mrep(P, out, mx, vals):
    P.S.emit("dve", lambda e: e.match_replace(out=out, in_to_replace=mx, in_values=vals, imm_value=-3.0e38), w=[out], r=[mx, vals])


def _top16_multi(P, items):
    for vals, vout, iout, tmp in items:
        _emit_max(P, vout[:, 0:8], vals)
    for vals, vout, iout, tmp in items:
        _emit_maxidx(P, iout[:, 0:8], vout[:, 0:8], vals)
    for vals, vout, iout, tmp in items:
        _emit_mrep(P, tmp, vout[:, 0:8], vals)
    for vals, vout, iout, tmp in items:
        _emit_max(P, vout[:, 8:16], tmp)
    for vals, vout, iout, tmp in items:
        _emit_maxidx(P, iout[:, 8:16], vout[:, 8:16], tmp)


def stage_peer_prep(P, l):
    I = P.inp
    m0 = P.top
    ub = [P.sb([128, DM], BF16) for _ in range(2)]
    uo = [P.sb([128, 8, 128], BF16) for _ in range(2)]
    vst = [P.sb([128, 4, DM], BF16) for _ in range(2)]
    pu = I["pu%d" % l].rearrange("(c i) d -> c i d", i=128)
    pv = I["pv%d" % l].rearrange("(c i) d -> i c d", i=128)
    vbv = P.vbS.rearrange("c i d -> i c d")
    for c in range(128):
        b = c % 2
        P.dma(ub[b], pu[c], q="pool")
        pT = P.bank(6 + b, 1024, BF16)
        for k in range(8):
            P.tr(pT[:, k * 128:(k + 1) * 128], ub[b][:, k * 128:(k + 1) * 128], P.ident_b)
        P.cp("act" if b == 0 else "dve", uo[b], pT.rearrange("p (k i) -> p k i", k=8))
        P.dma(P.utS[c], uo[b])
        if c % 4 == 0:
            vb_ = vst[(c // 4) % 2]
            P.dma(vb_, pv[:, c:c + 4, :], q="pool")
            P.dma(vbv[:, c:c + 4, :], vb_)
    P.top = m0


def stage_peer(P, l, xsrc, xdst, final):
    I = P.inp
    mark = P.top
    skT = P.sb([128, 16, 128], F32)
    P.dma(skT, I["skT%d" % l])
    W_sb = P.sb([128, 256, 128], BF16)
    h2T = P.sb([128, 8, 256], BF16)
    xt2 = [P.sb([128, DM], F32) for _ in range(2)]
    uTb = [P.sb([128, 2, 8, 128], BF16) for _ in range(2)]
    vbb = [P.sb([128, 2, DM], BF16) for _ in range(2)]
    utv = P.utS.rearrange("c p k i -> p c k i")
    vbv2 = P.vbS.rearrange("c i d -> i c d")
    I3T = P.sb([128, 3, 256], F32)
    Gb = [P.sb([128, 256], BF16) for _ in range(2)]
    Wa = [P.sb([128, 256], BF16) for _ in range(3)]
    etmp = [P.sb([128, 512], F32) for _ in range(2)]
    if final:
        fng = P.sb([128, DM], F32)
        P.dma(fng, I["fng"].partition_broadcast(128), q="pool")
        ot = [P.sb([128, DM], F32) for _ in range(2)]
    for gi in range(8):
        if "peer_prep_only" in P.debug or ("peer_1group" in P.debug and gi >= 1):
            break
        t0 = gi * 256
        for t2 in range(2):
            P.dma(xt2[t2], xsrc[t0 + t2 * 128: t0 + (t2 + 1) * 128, :])
            P.norm_tile(xt2[t2], P.A2, P.modT[:, 24:32], h2T[:, :, t2 * 128:(t2 + 1) * 128], 6 + t2)
        mB = P.top
        sc_sb = P.sb([128, 2, 16, 128], F32)
        mB2 = P.top
        wqb = [P.sb([128, 8, 128], BF16) for _ in range(2)]
        qf = [P.sb([128, 256], F32) for _ in range(2)]
        sq = [P.sb([128, 256], F32) for _ in range(2)]
        rs = [P.sb([128, 256], F32) for _ in range(2)]
        qn = [P.sb([128, 256], F32) for _ in range(2)]
        def q_s1(j):
            b = j % 2
            P.dma(wqb[b], I["wq%d" % l][:, :, j * 128:(j + 1) * 128], q="pool")
            q_ps = P.bank(2 + b)[:, 0:256]
            for k in range(8):
                P.mm(q_ps, wqb[b][:, k, :], h2T[:, k, :], start=(k == 0), stop=(k == 7))
            P.cp("act", qf[b], q_ps)
            P.act(sq[b], q_ps, AF.Square)

        def q_s2(j):
            b = j % 2
            s_ps = P.bank(4 + b)[:, 0:256]
            P.mm(s_ps, P.ones_f, sq[b])
            P.ts("dve", rs[b], s_ps, float(1.0 / 128), ALU.mult, 1.0e-6, ALU.add)
            P.act(rs[b], rs[b], AF.Sqrt)
            P.recip(rs[b], rs[b])
            P.tt("dve", qn[b], qf[b], rs[b], ALU.mult)
            c_ps = P.bank(0 + b)[:, 0:256]
            for t2 in range(2):
                P.mm(c_ps[:, t2 * 128:(t2 + 1) * 128], qn[b][:, t2 * 128:(t2 + 1) * 128], skT[:, j, :])
            P.cp("dve", sc_sb[:, :, j, :], c_ps.rearrange("p (a n) -> p a n", a=2))

        q_s1(0)
        for j in range(16):
            if j + 1 < 16:
                q_s1(j + 1)
            q_s2(j)
        if ("peer_sc%d" % l) in P.debug and gi == 0:
            dd = P.dram("dbg_peer_sc%d" % l, [128, 2, 16, 128], F32)
            P.dma(dd, sc_sb)
        P.top = mB2
        V = P.sb([128, 16, 16], F32)
        IDX = P.sb([128, 16, 16], U32)
        IDXf = P.sb([128, 16, 16], F32)
        tmpa = [P.sb([128, 128], F32) for _ in range(4)]
        cand = P.sb([128, 8, 256], F32)
        tmpc = [P.sb([128, 256], F32) for _ in range(4)]
        TOP = P.sb([128, 8, 16], F32)
        CI = P.sb([128, 8, 16], U32)
        AKu = P.sb([128, 8, 16], U32)
        BKu = P.sb([128, 8, 16], U32)
        AK = P.sb([128, 8, 16], F32)
        BK = P.sb([128, 8, 16], F32)
        oh = P.sb([128, 128, 16], F32)
        I1S = P.sb([128, 128], F32)
        I2S = P.sb([128, 128], F32)
        dsm = P.sb([128, 8, 16], F32)
        ssum = P.sb([128, 8], F32)
        Gm = P.sb([128, 8, 16], F32)
        iota16 = P.iota[:, 0:16].unsqueeze(1).unsqueeze(1).to_broadcast([128, 8, 16, 16])
        for t2 in range(2):
            if "peer_notopk" in P.debug:
                break
            for j0 in range(0, 16, 4):
                _top16_multi(P, [(sc_sb[:, t2, j, :], V[:, j, :], IDX[:, j, :], tmpa[j % 4]) for j in range(j0, j0 + 4)])
            P.cp("dve", IDXf, IDX)
            V4 = V.rearrange("p (h two) a -> p h two a", two=2)
            P.tt("dve", cand.rearrange("p h (a b) -> p h a b", a=16),
                 V4[:, :, 0, :].unsqueeze(3).to_broadcast([128, 8, 16, 16]),
                 V4[:, :, 1, :].unsqueeze(2).to_broadcast([128, 8, 16, 16]), ALU.add)
            for h0 in range(0, 8, 4):
                _top16_multi(P, [(cand[:, h, :], TOP[:, h, :], CI[:, h, :], tmpc[h % 4]) for h in range(h0, h0 + 4)])
            P.ts("dve", AKu, CI, 4, ALU.logical_shift_right)
            P.ts("dve", BKu, CI, 15, ALU.bitwise_and)
            P.cp("dve", AK, AKu)
            P.cp("dve", BK, BKu)
            IDX4 = IDXf.rearrange("p (h two) a -> p h two a", two=2)
            oh4 = oh.rearrange("p (h k) a -> p h k a", h=8)
            for XK, pidx, OUT in ((AK, 0, I1S), (BK, 1, I2S)):
                P.tt("dve", oh4, iota16, XK.unsqueeze(3).to_broadcast([128, 8, 16, 16]), ALU.is_equal)
                P.tt("dve", oh4, oh4, IDX4[:, :, pidx, :].unsqueeze(2).to_broadcast([128, 8, 16, 16]), ALU.mult)
                P.red("dve", OUT, oh, ALU.add)
            P.tt("dve", dsm, TOP, TOP[:, :, 0:1].to_broadcast([128, 8, 16]), ALU.subtract)
            P.act(dsm, dsm, AF.Exp)
            P.red("dve", ssum, dsm, ALU.add)
            P.recip(ssum, ssum)
            P.tt("dve", Gm, dsm, ssum.unsqueeze(2).to_broadcast([128, 8, 16]), ALU.mult)
            tr_ps = P.bank(6)[:, 0:384]
            P.tr(tr_ps[:, 0:128], I1S, P.ident_f)
            P.tr(tr_ps[:, 128:256], I2S, P.ident_f)
            P.tr(tr_ps[:, 256:384], Gm.rearrange("p h k -> p (h k)"), P.ident_f)
            P.cp("act", I3T[:, :, t2 * 128:(t2 + 1) * 128], tr_ps.rearrange("p (a t) -> p a t", a=3))
        if ("peer_sel%d" % l) in P.debug and gi == 0:
            dd = P.dram("dbg_peer_sel%d" % l, [128, 3, 256], F32)
            P.dma(dd, I3T)
        P.top = mB
        mC = P.top
        A = [P.sb([128, 16, 128], BF16) for _ in range(2)]
        B = [P.sb([128, 16, 128], BF16) for _ in range(2)]
        def ab_build(sbi):
            ab = sbi % 2
            tsl = slice(sbi * 16, sbi * 16 + 16)
            P.tt("dve", B[ab], P.iota.unsqueeze(1).to_broadcast([128, 16, 128]),
                 I3T[:, 1, tsl].unsqueeze(2).to_broadcast([128, 16, 128]), ALU.is_equal)
            for tl in range(16):
                t = sbi * 16 + tl
                P.ts("dve", A[ab][:, tl, :], P.iota, I3T[:, 0, t:t + 1], ALU.is_equal, I3T[:, 2, t:t + 1], ALU.mult)

        NSB = 0 if "peer_nowb" in P.debug else 16
        if NSB:
            ab_build(0)
        for sbi in range(NSB):
            ab = sbi % 2
            for q4 in range(4):
                wb_ = q4
                w_ps = P.bank(wb_)
                for u in range(4):
                    tl = q4 * 4 + u
                    P.mm(w_ps[:, u * 128:(u + 1) * 128], B[ab][:, tl, :], A[ab][:, tl, :])
            if sbi + 1 < NSB:
                ab_build(sbi + 1)
            for q4 in range(4):
                wb_ = q4
                w_ps = P.bank(wb_)
                tb = sbi * 16 + q4 * 4
                P.cp("act" if wb_ % 2 == 0 else "dve", W_sb[:, tb:tb + 4, :],
                     w_ps.rearrange("p (t c) -> p t c", t=4))
        P.top = mC
        SB3 = (4, 5, 6)

        def s_mm(c):
            if c % 2 == 0:
                P.dma(uTb[(c // 2) % 2], utv[:, c:c + 2])
                P.dma(vbb[(c // 2) % 2], vbv2[:, c:c + 2, :])
            ub_ = uTb[(c // 2) % 2][:, c % 2]
            S_ps = P.bank(SB3[c % 3])[:, 0:256]
            for k in range(8):
                P.mm(S_ps, ub_[:, k, :], h2T[:, k, :], start=(k == 0), stop=(k == 7))

        NC_ = 0 if "peer_nomain" in P.debug else 128
        if NC_:
            s_mm(0)
        for c in range(NC_):
            if c + 1 < NC_:
                s_mm(c + 1)
            vb_ = vbb[(c // 2) % 2][:, c % 2]
            S_ps = P.bank(SB3[c % 3])[:, 0:256]
            P.act(Gb[c % 2], S_ps, AF.Gelu)
            wa = Wa[c % 3]
            P.tt("dve" if c % 2 == 0 else "pool", wa, Gb[c % 2], W_sb[:, :, c], ALU.mult)
            for t2 in range(2):
                for half in range(2):
                    P.mm(P.bank(t2 * 2 + half), wa[:, t2 * 128:(t2 + 1) * 128], vb_[:, half * 512:(half + 1) * 512],
                         start=(c == 0), stop=(c == 127))
        for t2 in range(2):
            x_ = xt2[t2]
            for half in range(2):
                hs = slice(half * 512, (half + 1) * 512)
                e_ = etmp[half]
                P.tt("dve", e_, P.gt2[:, hs], P.bank(t2 * 2 + half), ALU.mult)
                P.tt("pool", x_[:, hs], x_[:, hs], e_, ALU.add)
            r0 = t0 + t2 * 128
            if not final:
                P.dma(xdst[r0:r0 + 128, :], x_)
            else:
                mF = P.top
                junk = P.sb([128, DM], BF16)
                ssq = P.sb([128, 1], F32)
                rstd = P.sb([128, 1], F32)
                P.act(junk, x_, AF.Square, accum=ssq)
                P.act(rstd, ssq, AF.Sqrt, scale=float(1.0 / DM), bias=P.eps_ap[:, 0:1])
                P.recip(rstd, rstd)
                o_ = ot[t2]
                P.ts("dve", o_, x_, rstd[:, 0:1], ALU.mult)
                P.tt("pool", o_, o_, fng, ALU.mult)
                P.dma(xdst[r0:r0 + 128, :], o_)
                P.top = mF
    P.top = mark


def kernel(**inputs):
    inp = {k: np.asarray(v) for k, v in inputs.items()}
    shared = _prep_shared(inp)
    cores = [core_inputs(inp, b) for b in range(NCORES)]
    P = build(input_shapes(shared, cores[0]), upto="all")
    in_maps = []
    for b in range(NCORES):
        m = dict(shared)
        m.update(cores[b])
        in_maps.append(m)
    res = run_bass_kernel_spmd(P.nc, in_maps, core_ids=list(range(NCORES)))
    out = np.stack([np.asarray(res.results[b]["out"], dtype=np.float32) for b in range(NCORES)], axis=0)
    return out
```
